# Optimizing a Trainium2 kernel written in Bass

```python
import jax
import jax.numpy as jnp
from jax import lax
import numpy as np

D_MODEL = 1024
BATCH = 4
SEQ = 4096
DEPTH = 2

GRID_W = 64
CTX_LEN = 256
NORM_EPS = 1e-6
N_MOD = 6

ATT_HEADS = 8
ATT_KV_HEADS = 2
ATT_GROUP = ATT_HEADS // ATT_KV_HEADS
HEAD_DIM = 128
ATT_WIDTH = ATT_HEADS * HEAD_DIM
KV_WIDTH = ATT_KV_HEADS * HEAD_DIM
Q_BLOCK = 128
ROPE_THETA = 10000.0
ROPE_FREQS_PER_AXIS = HEAD_DIM // 4

HGRN_EXPAND = 128
HGRN_HEADS = D_MODEL // HGRN_EXPAND
HGRN_DV = D_MODEL // HGRN_HEADS
HGRN_WIDTH = HGRN_HEADS * HGRN_EXPAND
HGRN_VWIDTH = HGRN_HEADS * HGRN_DV
HGRN_CHUNK = 64

PROJ_SIZES = (ATT_WIDTH, KV_WIDTH, KV_WIDTH, HGRN_WIDTH, HGRN_WIDTH, HGRN_WIDTH,
              HGRN_VWIDTH, HGRN_VWIDTH, D_MODEL, D_MODEL)
PROJ_WIDTH = sum(PROJ_SIZES)
PROJ_SPLITS = tuple(int(v) for v in np.cumsum(PROJ_SIZES)[:-1])

D_FF_DENSE = 256 * ((8 * D_MODEL // 3 + 255) // 256)
N_EXPERTS = 8
TOP_K = 2
D_FF_EXPERT = 7 * D_MODEL // 2
MOE_BLOCK = 256
N_DENSE_LAYERS = (DEPTH + 1) // 2
N_MOE_LAYERS = DEPTH // 2

kernel_name = 'hybrid_gqa_hgrn2_moe_dit_prefix'


def rms_norm(x, gain):
    xf = x.astype(jnp.float32)
    y = xf * lax.rsqrt(jnp.mean(xf * xf, axis=-1, keepdims=True) + NORM_EPS)
    return (y * gain.astype(jnp.float32)).astype(x.dtype)


def modulate(h, shift, scale):
    return h * (1.0 + scale) + shift


def split_heads(t, n_heads):
    return t.reshape(*t.shape[:-1], n_heads, t.shape[-1] // n_heads)


def axial_rope_tables(n_tokens):
    rows = n_tokens // GRID_W
    row_ids = jnp.repeat(jnp.arange(rows), GRID_W).astype(jnp.float32)
    col_ids = jnp.tile(jnp.arange(GRID_W), rows).astype(jnp.float32)
    inv_freq = ROPE_THETA ** (-jnp.arange(ROPE_FREQS_PER_AXIS, dtype=jnp.float32) / ROPE_FREQS_PER_AXIS)
    ang = jnp.concatenate([row_ids[:, None] * inv_freq, col_ids[:, None] * inv_freq], axis=-1)
    ang = jnp.concatenate([ang, ang], axis=-1)
    return jnp.cos(ang), jnp.sin(ang)


def apply_rope(x, cos, sin):
    half = x.shape[-1] // 2
    xf = x.astype(jnp.float32)
    rot = jnp.concatenate([-xf[..., half:], xf[..., :half]], axis=-1)
    return (xf * cos[None, :, None, :] + rot * sin[None, :, None, :]).astype(x.dtype)


def gqa_softmax(q, k, v):
    s = jnp.einsum('bqkgd,bskd->bkgqs', q, k).astype(jnp.float32) * (HEAD_DIM ** -0.5)
    p = jax.nn.softmax(s, axis=-1).astype(v.dtype)
    return jnp.einsum('bkgqs,bskd->bqkgd', p, v)


def latent_attention(q, k_lat, v_lat, k_ctx, v_ctx):
    b, n = q.shape[:2]
    k_all = jnp.concatenate([k_ctx, k_lat], axis=1)
    v_all = jnp.concatenate([v_ctx, v_lat], axis=1)
    qb = q.reshape(b, n // Q_BLOCK, Q_BLOCK, ATT_KV_HEADS, ATT_GROUP, HEAD_DIM).transpose(1, 0, 2, 3, 4, 5)
    ob = lax.map(lambda blk: gqa_softmax(blk, k_all, v_all), qb)
    return ob.transpose(1, 0, 2, 3, 4, 5).reshape(b, n, ATT_WIDTH)


def context_attention(q, k_ctx, v_ctx):
    b, l = q.shape[:2]
    qg = q.reshape(b, l, ATT_KV_HEADS, ATT_GROUP, HEAD_DIM)
    return gqa_softmax(qg, k_ctx, v_ctx).reshape(b, l, ATT_WIDTH)


def hgrn_lower_bounds(logits):
    lb = jnp.cumsum(jax.nn.softmax(logits.astype(jnp.float32), axis=0), axis=0)
    return lb - lb[0:1]


def hgrn_forget(f_pre, lb):
    f = lb + (1.0 - lb) * jax.nn.sigmoid(f_pre.astype(jnp.float32))
    return split_heads(1.0 - f, HGRN_HEADS), split_heads(jnp.log(f), HGRN_HEADS)


def hgrn2_chunk_scan(q, k, v, log_f, state0):
    b, n, h, _ = q.shape
    dv = v.shape[-1]
    nc = n // HGRN_CHUNK

    def to_chunks(t):
        return t.astype(jnp.float32).reshape(b, nc, HGRN_CHUNK, h, t.shape[-1]).transpose(1, 0, 3, 2, 4)

    causal = jnp.tril(jnp.ones((HGRN_CHUNK, HGRN_CHUNK), dtype=bool))[:, :, None]

    def step(state, chunk):
        qc, kc, vc, gc = chunk
        cum = jnp.cumsum(gc, axis=2)
        rel = jnp.where(causal, cum[:, :, :, None, :] - cum[:, :, None, :, :], -jnp.inf)
        scores = jnp.einsum('bhtd,bhsd,bhtsd->bhts', qc, kc, jnp.exp(rel))
        out = (jnp.einsum('bhts,bhsv->bhtv', scores, vc)
               + jnp.einsum('bhtd,bhdv->bhtv', qc * jnp.exp(cum), state))
        tail = cum[:, :, -1:, :]
        new_state = (jnp.exp(tail[:, :, 0, :, None]) * state
                     + jnp.einsum('bhsd,bhsv->bhdv', kc * jnp.exp(tail - cum), vc))
        return new_state, out

    final, outs = lax.scan(step, state0, tuple(to_chunks(t) for t in (q, k, v, log_f)))
    o = outs.transpose(1, 0, 3, 2, 4).reshape(b, n, h, dv)
    return o.astype(v.dtype), final


def hgrn_readout(o, gate, gain):
    o = rms_norm(o, gain)
    return o.reshape(*o.shape[:2], HGRN_VWIDTH) * jax.nn.silu(gate)


def merge_branches(att_o, hg_o, gate_att, gate_hg, w_att_br, w_hg_br, w_out_l):
    y = (jax.nn.sigmoid(gate_att) * (att_o @ w_att_br)
         + jax.nn.sigmoid(gate_hg) * (hg_o @ w_hg_br))
    return y @ w_out_l


def swiglu(t, w_gate, w_up, w_down):
    return (jax.nn.silu(t @ w_gate) * (t @ w_up)) @ w_down


def moe_swiglu(t, w_router, w_gate, w_up, w_down):
    n_tok, d = t.shape
    logits = (t @ w_router).astype(jnp.float32)
    top_logits, top_idx = lax.top_k(logits, TOP_K)
    top_w = jax.nn.softmax(top_logits, axis=-1).astype(t.dtype)
    flat_e = top_idx.reshape(-1)
    n_assign = n_tok * TOP_K
    order = jnp.argsort(flat_e)
    sorted_e = flat_e[order]
    counts = jnp.bincount(flat_e, length=N_EXPERTS)
    padded = (counts + MOE_BLOCK - 1) // MOE_BLOCK * MOE_BLOCK
    start = jnp.cumsum(counts) - counts
    pend = jnp.cumsum(padded)
    pstart = pend - padded
    dest_sorted = (pstart[sorted_e] + jnp.arange(n_assign) - start[sorted_e]).astype(jnp.int32)
    n_blocks = -(-n_assign // MOE_BLOCK) + N_EXPERTS
    n_rows = n_blocks * MOE_BLOCK
    row_token = jnp.full((n_rows,), n_tok, jnp.int32).at[dest_sorted].set((order // TOP_K).astype(jnp.int32))
    block_expert = jnp.minimum(
        jnp.searchsorted(pend, jnp.arange(n_blocks) * MOE_BLOCK, side='right'), N_EXPERTS - 1)
    t_pad = jnp.concatenate([t, jnp.zeros((1, d), t.dtype)], axis=0)
    xb = t_pad[row_token].reshape(n_blocks, MOE_BLOCK, d)

    def expert_block(args):
        xblk, e = args
        return swiglu(xblk, w_gate[e], w_up[e], w_down[e])

    yb = lax.map(expert_block, (xb, block_expert)).reshape(n_rows, d)
    dest = jnp.zeros((n_assign,), jnp.int32).at[order].set(dest_sorted)
    y_sel = yb[dest].reshape(n_tok, TOP_K, d)
    return jnp.einsum('tk,tkd->td', top_w, y_sel)


def setup_inputs(seed: int = 0) -> dict:
    key = jax.random.key(seed)
    ks = iter(jax.random.split(key, 32))
    D = D_MODEL

    def nrm(shape, scale):
        return scale * jax.random.normal(next(ks), shape, jnp.float32)

    def gain(shape):
        return 1.0 + nrm(shape, 0.02)

    return {
        'x': nrm((BATCH, SEQ, D), 1.0),
        'c': nrm((BATCH, D), 1.0),
        'ctx': nrm((BATCH, CTX_LEN, D), 1.0),
        'c_ctx': nrm((D,), 1.0),
        'w_mod': nrm((DEPTH, D, N_MOD * D), 0.5 * D ** -0.5),
        'b_mod': nrm((DEPTH, N_MOD * D), 0.02),
        'pre_mix_norm': gain((DEPTH, D)),
        'post_mix_norm': gain((DEPTH, D)),
        'pre_ffn_norm': gain((DEPTH, D)),
        'post_ffn_norm': gain((DEPTH, D)),
        'w_in': nrm((DEPTH, D, PROJ_WIDTH), D ** -0.5),
        'q_norm': gain((DEPTH, HEAD_DIM)),
        'k_norm': gain((DEPTH, HEAD_DIM)),
        'hg_norm': gain((DEPTH, HGRN_DV)),
        'hg_lb_logits': nrm((DEPTH, HGRN_WIDTH), 1.0),
        'w_att_branch': nrm((DEPTH, ATT_WIDTH, D), ATT_WIDTH ** -0.5),
        'w_hg_branch': nrm((DEPTH, HGRN_VWIDTH, D), HGRN_VWIDTH ** -0.5),
        'w_out': nrm((DEPTH, D, D), D ** -0.5),
        'ffn_w_gate': nrm((N_DENSE_LAYERS, D, D_FF_DENSE), D ** -0.5),
        'ffn_w_up': nrm((N_DENSE_LAYERS, D, D_FF_DENSE), D ** -0.5),
        'ffn_w_down': nrm((N_DENSE_LAYERS, D_FF_DENSE, D), D_FF_DENSE ** -0.5),
        'moe_router': nrm((N_MOE_LAYERS, D, N_EXPERTS), D ** -0.5),
        'moe_w_gate': nrm((N_MOE_LAYERS, N_EXPERTS, D, D_FF_EXPERT), D ** -0.5),
        'moe_w_up': nrm((N_MOE_LAYERS, N_EXPERTS, D, D_FF_EXPERT), D ** -0.5),
        'moe_w_down': nrm((N_MOE_LAYERS, N_EXPERTS, D_FF_EXPERT, D), D_FF_EXPERT ** -0.5),
    }


def reference(x, c, ctx, c_ctx, w_mod, b_mod, pre_mix_norm, post_mix_norm, pre_ffn_norm,
              post_ffn_norm, w_in, q_norm, k_norm, hg_norm, hg_lb_logits, w_att_branch,
              w_hg_branch, w_out, ffn_w_gate, ffn_w_up, ffn_w_down, moe_router, moe_w_gate,
              moe_w_up, moe_w_down):
    b, n, d = x.shape
    l_ctx = ctx.shape[1]
    cos, sin = axial_rope_tables(n)
    lower_bounds = hgrn_lower_bounds(hg_lb_logits)
    zero_state = jnp.zeros((b, HGRN_HEADS, HGRN_EXPAND, HGRN_DV), jnp.float32)
    x_lat, x_ctx = x, ctx

    for layer in range(DEPTH):
        last = layer == DEPTH - 1
        mod_lat = (jax.nn.silu(c) @ w_mod[layer] + b_mod[layer])[:, None, :]
        mod_ctx = (jax.nn.silu(c_ctx) @ w_mod[layer] + b_mod[layer])[None, None, :]
        sh1, sc1, g1, sh2, sc2, g2 = jnp.split(mod_lat, N_MOD, axis=-1)
        csh1, csc1, cg1, csh2, csc2, cg2 = jnp.split(mod_ctx, N_MOD, axis=-1)
        lb = lower_bounds[layer]

        h_ctx = modulate(rms_norm(x_ctx, pre_mix_norm[layer]), csh1, csc1)
        h_lat = modulate(rms_norm(x_lat, pre_mix_norm[layer]), sh1, sc1)
        (aq_c, ak_c, av_c, hq_c, hff_c, hfb_c, hi_c, hg_c, ga_c, gh_c) = jnp.split(
            h_ctx @ w_in[layer], PROJ_SPLITS, axis=-1)
        (aq_l, ak_l, av_l, hq_l, hff_l, hfb_l, hi_l, hg_l, ga_l, gh_l) = jnp.split(
            h_lat @ w_in[layer], PROJ_SPLITS, axis=-1)

        k_ctx = rms_norm(split_heads(ak_c, ATT_KV_HEADS), k_norm[layer])
        v_ctx = split_heads(av_c, ATT_KV_HEADS)
        hq_ctx = split_heads(jax.nn.silu(hq_c), HGRN_HEADS)
        hv_ctx = split_heads(hi_c, HGRN_HEADS)
        kf_c, lf_c = hgrn_forget(hff_c, lb)
        kb_c, lb_c = hgrn_forget(hfb_c, lb)
        o_cf, state_f = hgrn2_chunk_scan(hq_ctx, kf_c, hv_ctx, lf_c, zero_state)
        o_cb, state_b = hgrn2_chunk_scan(jnp.flip(hq_ctx, 1), jnp.flip(kb_c, 1), jnp.flip(hv_ctx, 1),
                                         jnp.flip(lb_c, 1), zero_state)

        q_lat = apply_rope(rms_norm(split_heads(aq_l, ATT_HEADS), q_norm[layer]), cos, sin)
        k_lat = apply_rope(rms_norm(split_heads(ak_l, ATT_KV_HEADS), k_norm[layer]), cos, sin)
        v_lat = split_heads(av_l, ATT_KV_HEADS)
        att_lat = latent_attention(q_lat, k_lat, v_lat, k_ctx, v_ctx)
        hq_lat = split_heads(jax.nn.silu(hq_l), HGRN_HEADS)
        hv_lat = split_heads(hi_l, HGRN_HEADS)
        kf_l, lf_l = hgrn_forget(hff_l, lb)
        kb_l, lb_l = hgrn_forget(hfb_l, lb)
        o_lf, _ = hgrn2_chunk_scan(hq_lat, kf_l, hv_lat, lf_l, state_f)
        o_lb, _ = hgrn2_chunk_scan(jnp.flip(hq_lat, 1), jnp.flip(kb_l, 1), jnp.flip(hv_lat, 1),
                                   jnp.flip(lb_l, 1), state_b)
        hg_lat = hgrn_readout(o_lf + jnp.flip(o_lb, 1), hg_l, hg_norm[layer])
        mix_lat = merge_branches(att_lat, hg_lat, ga_l, gh_l, w_att_branch[layer],
                                 w_hg_branch[layer], w_out[layer])
        x_lat = x_lat + g1 * rms_norm(mix_lat, post_mix_norm[layer])

        if not last:
            q_ctx = rms_norm(split_heads(aq_c, ATT_HEADS), q_norm[layer])
            att_ctx = context_attention(q_ctx, k_ctx, v_ctx)
            hg_ctx = hgrn_readout(o_cf + jnp.flip(o_cb, 1), hg_c, hg_norm[layer])
            mix_ctx = merge_branches(att_ctx, hg_ctx, ga_c, gh_c, w_att_branch[layer],
                                     w_hg_branch[layer], w_out[layer])
            x_ctx = x_ctx + cg1 * rms_norm(mix_ctx, post_mix_norm[layer])

        f_lat = modulate(rms_norm(x_lat, pre_ffn_norm[layer]), sh2, sc2).reshape(b * n, d)
        if not last:
            f_ctx = modulate(rms_norm(x_ctx, pre_ffn_norm[layer]), csh2, csc2).reshape(b * l_ctx, d)
            tokens = jnp.concatenate([f_lat, f_ctx], axis=0)
        else:
            tokens = f_lat
        idx = layer // 2
        if layer % 2 == 0:
            y = swiglu(tokens, ffn_w_gate[idx], ffn_w_up[idx], ffn_w_down[idx])
        else:
            y = moe_swiglu(tokens, moe_router[idx], moe_w_gate[idx], moe_w_up[idx], moe_w_down[idx])
        y = rms_norm(y, post_ffn_norm[layer])
        x_lat = x_lat + g2 * y[:b * n].reshape(b, n, d)
        if not last:
            x_ctx = x_ctx + cg2 * y[b * n:].reshape(b, l_ctx, d)

    return x_lat
```

```python
from contextlib import ExitStack

import numpy as np
import concourse.bass as bass
import concourse.mybir as mybir
from concourse.bass_utils import run_bass_kernel_spmd

F32 = mybir.dt.float32
BF16 = mybir.dt.bfloat16
U32 = mybir.dt.uint32
AF = mybir.ActivationFunctionType
ALU = mybir.AluOpType
AX = mybir.AxisListType

D = 1024
NLAT = 2048
NCTX = 256
NTOK = NLAT + NCTX
NKEY = NTOK + NLAT
PW = 8704
EPS = 1e-6
DFF = 2816
DFE = 3584
NEXP = 8
BLK_TYPES = ["q", "q", "kv", "hq", "hq", "fA", "fA", "fB", "fB", "hi", "hi", "hg", "hg",
             "ga", "ga", "gh", "gh"]


class Buf:
    def __init__(self, prog, ap, name, dram=False):
        self.p = prog
        self.ap = ap
        self.name = name
        self.dram = dram
        self.w = {}
        self.r = {}
        self.dsem = None

    def __getitem__(self, k):
        return self.ap[k]


class _View:
    def __init__(self, buf, ap):
        self.buf = buf
        self.ap = ap

    def __getitem__(self, k):
        return self.ap[k]


class Eng:
    def __init__(self, prog, e, name, fifo=False):
        self.p = prog
        self.e = e
        self.name = name
        self.sem = prog.nc.alloc_semaphore("tl_" + name)
        self.n = 0
        self.seen = {}
        self.fifo = fifo

    def wait(self, sem, val):
        if sem is self.sem and self.fifo:
            return
        k = id(sem)
        if self.seen.get(k, 0) >= val:
            return
        self.e.wait_ge(sem, val)
        self.seen[k] = val


class Prog:
    def __init__(self):
        nc = bass.Bass("TRN2", target_bir_lowering=False)
        self.nc = nc
        self.pe = Eng(self, nc.tensor, "pe", fifo=True)
        self.act = Eng(self, nc.scalar, "act")
        self.dve = Eng(self, nc.vector, "dve")
        self.pool = Eng(self, nc.gpsimd, "pool")
        self.sp = Eng(self, nc.sync, "sp")
        self.engs = [self.pe, self.act, self.dve, self.pool, self.sp]
        self.dma_pool = []
        self.dma_pools = {}
        self.dma_all = []
        self.nbuf = 0
        self.stack = None
        self.local = []
        self.scopes = []

    def sb(self, shape, dt, name=None):
        self.nbuf += 1
        name = (name or "sb") + "_%d" % self.nbuf
        if self.stack is not None:
            t = self.stack.enter_context(self.nc.sbuf_tensor(name, list(shape), dt))
            b = Buf(self, t[:], name)
            self.local.append(b)
            return b
        t = self.nc.alloc_sbuf_tensor(name, list(shape), dt)
        return Buf(self, t[:], name)

    def begin(self):
        self.scopes.append((self.stack, self.local))
        self.stack = ExitStack()
        self.local = []

    def end(self):
        self.barrier()
        self.release(self.local)
        self.stack.close()
        self.stack, self.local = self.scopes.pop()

    def dram(self, name, shape, dt, kind="Internal"):
        t = self.nc.dram_tensor(name, list(shape), dt, kind=kind)
        return Buf(self, t.ap(), name, dram=True)

    def sub(self, ap, name="sub"):
        return Buf(self, ap, name)

    def _dsem(self, buf, kind):
        if buf.dsem is None:
            buf.dsem = {}
        if kind not in buf.dsem:
            pool = self.dma_pools.setdefault(kind, [])
            if pool:
                buf.dsem[kind] = pool.pop()
            else:
                ent = [self.nc.alloc_semaphore("dm%s%d" % (kind, len(self.dma_all))), 0]
                self.dma_all.append(ent)
                buf.dsem[kind] = ent
        return buf.dsem[kind]

    def release(self, bufs):
        for b in bufs:
            if b.dsem:
                for kind, ent in b.dsem.items():
                    self.dma_pools.setdefault(kind, []).append(ent)
                b.dsem = None

    def _deps(self, eng, reads, writes):
        for b in reads:
            if b.dram:
                continue
            for (s, v) in b.w.values():
                eng.wait(s, v)
        for b in writes:
            if b.dram:
                continue
            for (s, v) in b.w.values():
                eng.wait(s, v)
            for (s, v) in b.r.values():
                eng.wait(s, v)

    def _record(self, tok, reads, writes):
        s, v = tok
        k = id(s)
        for b in reads:
            o = b.r.get(k)
            if o is None or o[1] < v:
                b.r[k] = (s, v)
        for b in writes:
            if b.dram:
                o = b.w.get(k)
                if o is None or o[1] < v:
                    b.w[k] = (s, v)
            else:
                b.w = {k: (s, v)}
                b.r = {}

    def op(self, eng, fn, reads=(), writes=(), inc=True):
        self._deps(eng, reads, writes)
        ins = fn(eng.e)
        if inc:
            eng.n += 1
            ins.then_inc(eng.sem, 1)
            tok = (eng.sem, eng.n)
        else:
            tok = (eng.sem, eng.n + 1)
        self._record(tok, reads, writes)
        return tok

    def dma(self, q, out_buf, out_ap, in_buf, in_ap):
        self._deps(q, [in_buf], [out_buf])
        sbside = in_buf if out_buf.dram else out_buf
        ent = self._dsem(sbside, "sw" if q is self.pool else "hw")
        ins = q.e.dma_start(out=out_ap, in_=in_ap)
        ent[1] += 16
        ins.then_inc(ent[0], 16)
        tok = (ent[0], ent[1])
        self._record(tok, [in_buf], [out_buf])
        return tok

    def barrier(self, bufs=()):
        toks = [(e.sem, e.n) for e in self.engs if e.n > 0]
        for ent in self.dma_all:
            if ent[1] > 0:
                toks.append((ent[0], ent[1]))
        for e in self.engs:
            for (s, v) in toks:
                if s is not e.sem:
                    e.wait(s, v)
        for b in bufs:
            b.w = {}
            b.r = {}


class Builder:
    def __init__(self, layers=(0, 1), stop_after=None, dump=(), moe=True, ctx_out=True):
        self.P = Prog()
        self.nc = self.P.nc
        self.layers = layers
        self.stop_after = stop_after
        self.dump = set(dump)
        P = self.P
        nc = self.nc
        self.in_names = []

        def ein(n, s, dt=F32):
            self.in_names.append(n)
            return P.dram(n, s, dt, kind="ExternalInput")
        self.xl = ein("xl", [NLAT, D])
        self.xp = ein("xp", [NLAT, D])
        self.xc = ein("xc", [NCTX, D])
        self.cT = ein("cT", [128, 16])
        self.cs_own = ein("cs_own", [NLAT, 256])
        self.cs_par = ein("cs_par", [NLAT, 256])
        self.consts = ein("consts", [128, 1024])
        self.w_mod = ein("w_mod", [2, D, 6 * D])
        self.colv = ein("colv", [2, 128, 48])
        self.rowv = ein("rowv", [2, 128, 4096 + 384 + 2048])
        self.w_in = ein("w_in", [2, D, PW])
        self.w_att = ein("w_att", [2, D, D])
        self.w_hg = ein("w_hg", [2, D, D])
        self.w_out = ein("w_out", [2, D, D])
        self.ffn_g = ein("ffn_g", [D, DFF])
        self.ffn_u = ein("ffn_u", [D, DFF])
        self.ffn_d = ein("ffn_d", [DFF, D])
        if moe:
            self.moe_r = ein("moe_r", [D, NEXP])
            self.moe_g = ein("moe_g", [NEXP, D, DFE])
            self.moe_u = ein("moe_u", [NEXP, D, DFE])
            self.moe_d = ein("moe_d", [NEXP, DFE, D])
        self.yout = P.dram("yout", [NLAT, D], F32, kind="ExternalOutput")
        self.xcout = P.dram("xcout", [NCTX, D], F32, kind="ExternalOutput") if ctx_out else None

        def scr(n, s, dt):
            return P.dram(n, s, dt, kind="ExternalOutput" if n in self.dump else "Internal")
        self.QT = scr("QT", [8, 128, NKEY], BF16)
        self.KT = scr("KT", [2, 128, NKEY], BF16)
        self.V = scr("V", [NKEY, 256], BF16)
        self.HQ = scr("HQ", [NKEY, D], BF16)
        self.KA = scr("KA", [NKEY, D], BF16)
        self.KB = scr("KB", [NKEY, D], BF16)
        self.LFA = scr("LFA", [NKEY, D], F32)
        self.LFB = scr("LFB", [NKEY, D], F32)
        self.HI = scr("HI", [NKEY, D], BF16)
        self.HG = scr("HG", [NKEY, D], BF16)
        self.GA = scr("GA", [NKEY, D], BF16)
        self.GH = scr("GH", [NKEY, D], BF16)
        self.OA = scr("OA", [NKEY, D], F32)
        self.OB = scr("OB", [NKEY, D], F32)
        self.ATT = scr("ATT", [8, 128, NKEY], BF16)
        self.AT = scr("AT", [22, 128, NKEY], BF16)
        if moe:
            self.ATE = scr("ATE", [NEXP, 28, 128, NLAT], BF16)
        self.X1 = scr("X1", [NKEY, D], F32)
        self.X2 = scr("X2", [NKEY, D], F32)
        self.HTD = scr("HTD", [128, 8, NKEY], BF16)

        self.bank = [Buf(P, nc.alloc_psum_tensor("bank%d" % i, [128, 512], F32)[:], "bank%d" % i)
                     for i in range(8)]

        self.c_f32 = P.sb([128, 1024], F32, "consts")
        self.ident_bf = P.sb([128, 128], BF16, "identbf")
        self.ones_bf = P.sb([128, 128], BF16, "onesbf")
        P.dma(P.sp, self.c_f32, self.c_f32[:], self.consts, self.consts[:])
        P.op(P.dve, lambda e: e.tensor_copy(out=self.ident_bf[:], in_=self.c_f32[:, 0:128]),
             [self.c_f32], [self.ident_bf])
        P.op(P.dve, lambda e: e.memset(self.ones_bf[:], 1.0), [], [self.ones_bf])
        self.ident_f = self.c_f32[:, 0:128]
        self.AB = P.sb([128, 4, 8, 2], F32, "AB")
        self.grow = P.sb([128, 2, 2, 1024], F32, "grow")
        self.hT = None

    def wblock_load(self, wbuf, src_ap):
        P = self.P
        return src_ap.rearrange("(kc k) j -> k kc j", k=128)

    def phase_M(self, layer):
        P = self.P
        P.begin()
        cT = P.sb([128, 8, 2], F32, "cTf")
        cs = P.sb([128, 8, 2], BF16, "csbf")
        crep = P.sb([128, 2, 8, 128], BF16, "crep")
        colv = P.sb([128, 48], F32, "colv")
        rows = P.sb([128, 4096], F32, "rows")
        wb = [P.sb([128, 8, 512], BF16, "wmod%d" % i) for i in range(3)]
        modc = P.sb([128, 4, 8, 2], F32, "modc")
        tmp = P.sb([128, 8, 2], F32, "tmpM")
        P.dma(P.sp, cT, cT[:], self.cT, self.cT[:].rearrange("p (k x) -> p k x", x=2))
        P.dma(P.sp, colv, colv[:], self.colv, self.colv[layer])
        P.dma(P.sp, rows, rows[:], self.rowv, self.rowv[layer][:, 0:4096])
        P.op(P.act, lambda e: e.activation(out=cs[:], in_=cT[:], func=AF.Silu), [cT], [cs])
        for x in range(2):
            P.op(P.dve, lambda e: e.tensor_copy(out=crep[:, x], in_=cs[:, :, x:x + 1].to_broadcast([128, 8, 128])),
                 [cs], [crep])
        psM = self.bank[0]
        psG = [self.bank[1], self.bank[2]]
        for cb in range(12):
            w = wb[cb % 3]
            src = self.w_mod[layer][:, cb * 512:(cb + 1) * 512].rearrange("(kc k) j -> k kc j", k=128)
            P.dma(P.pool, w, w[:], self.w_mod, src)
            slot, half = cb // 2, cb % 2
            if slot in (0, 1, 3, 4):
                sidx = {0: 1, 1: 0, 3: 3, 4: 2}[slot]
                for j in range(4):
                    col = (sidx * 8 + half * 4 + j) * 2
                    for kc in range(8):
                        P.op(P.pe, lambda e: e.matmul(psM[:, col:col + 2], w[:, kc, j * 128:(j + 1) * 128],
                                                      cs[:, kc, :], start=(kc == 0), stop=(kc == 7)),
                             [w, cs], [psM], inc=(kc == 7))
            else:
                gi = 0 if slot == 2 else 1
                for x in range(2):
                    for kc in range(8):
                        P.op(P.pe, lambda e: e.matmul(psG[x][:], crep[:, x, kc, :], w[:, kc, :],
                                                      start=(kc == 0), stop=(kc == 7)),
                             [w, crep], [psG[x]], inc=(kc == 7))
                    dst = self.grow[:, x, gi, half * 512:(half + 1) * 512]
                    P.op(P.dve, lambda e: e.tensor_tensor(out=dst, in0=psG[x][:],
                                                          in1=rows[:, gi * 1024 + half * 512: gi * 1024 + (half + 1) * 512],
                                                          op=ALU.add), [psG[x], rows], [self.grow])
                    P.op(P.dve, lambda e: e.tensor_tensor(out=dst, in0=dst,
                                                          in1=rows[:, (2 + gi) * 1024 + half * 512: (2 + gi) * 1024 + (half + 1) * 512],
                                                          op=ALU.mult), [rows, self.grow], [self.grow])
        P.op(P.dve, lambda e: e.tensor_tensor(
            out=modc[:], in0=psM[:, 0:64].rearrange("p (s k x) -> p s k x", s=4, k=8),
            in1=colv[:, 16:48].rearrange("p (s k) -> p s k", s=4).unsqueeze(3).to_broadcast([128, 4, 8, 2]),
            op=ALU.add), [psM, colv], [modc])
        for i in range(2):
            P.op(P.dve, lambda e: e.tensor_scalar(out=tmp[:], in0=modc[:, 2 * i], scalar1=1.0, scalar2=None,
                                                  op0=ALU.add), [modc], [tmp])
            P.op(P.dve, lambda e: e.tensor_tensor(out=self.AB[:, 2 * i], in0=tmp[:],
                                                  in1=colv[:, 8 * i:8 * i + 8].unsqueeze(2).to_broadcast([128, 8, 2]),
                                                  op=ALU.mult), [tmp, colv], [self.AB])
            P.op(P.dve, lambda e: e.tensor_copy(out=self.AB[:, 2 * i + 1], in_=modc[:, 2 * i + 1]),
                 [modc], [self.AB])
        P.end()

    def phase_H(self, tiles, which):
        P = self.P
        P.begin()
        xt = [P.sb([128, D], F32, "xt%d" % i) for i in range(3)]
        xs = [P.sb([128, D], BF16, "xs%d" % i) for i in range(2)]
        junk = P.sb([128, D], BF16, "junk")
        ss = [P.sb([128, 2], F32, "ss%d" % i) for i in range(2)]
        psT = [self.bank[0], self.bank[1]]

        def stage_a(n):
            src, row0, xidx, col = tiles[n]
            a = xt[n % 3]
            b = xs[n % 2]
            s_ = ss[n % 2]
            P.op(P.act, lambda e: e.activation(out=junk[:], in_=a[:], func=AF.Square, accum_out=s_[:, 0:1]),
                 [a], [junk, s_])
            P.op(P.act, lambda e: e.activation(out=s_[:, 1:2], in_=s_[:, 0:1], func=AF.Sqrt, scale=1.0 / D, bias=EPS),
                 [s_], [s_])
            P.op(P.dve, lambda e: e.reciprocal(out=s_[:, 0:1], in_=s_[:, 1:2]), [s_], [s_])
            P.op(P.dve, lambda e: e.tensor_scalar(out=b[:], in0=a[:], scalar1=s_[:, 0:1], scalar2=None, op0=ALU.mult),
                 [a, s_], [b])

        def stage_b(n):
            src, row0, xidx, col = tiles[n]
            b = xs[n % 2]
            pt = psT[n % 2]
            ptb = pt[:].bitcast(BF16)
            for kc in range(8):
                P.op(P.pe, lambda e: e.transpose(out=ptb[:, kc * 128:(kc + 1) * 128], in_=b[:, kc * 128:(kc + 1) * 128],
                                                 identity=self.ident_bf[:]), [b, self.ident_bf], [pt], inc=(kc == 7))
            for kc in range(8):
                eng = P.dve
                if eng is P.act:
                    P.op(eng, lambda e: e.activation(out=self.hT[:, kc, col:col + 128], in_=ptb[:, kc * 128:(kc + 1) * 128],
                                                     func=AF.Identity, scale=self.AB[:, 2 * which, kc, xidx:xidx + 1],
                                                     bias=self.AB[:, 2 * which + 1, kc, xidx:xidx + 1]),
                         [pt, self.AB], [])
                else:
                    P.op(eng, lambda e: e.tensor_scalar(out=self.hT[:, kc, col:col + 128], in0=ptb[:, kc * 128:(kc + 1) * 128],
                                                        scalar1=self.AB[:, 2 * which, kc, xidx:xidx + 1],
                                                        scalar2=self.AB[:, 2 * which + 1, kc, xidx:xidx + 1],
                                                        op0=ALU.mult, op1=ALU.add),
                         [pt, self.AB], [])

        def load(n):
            src, row0, xidx, col = tiles[n]
            P.dma(P.sp, xt[n % 3], xt[n % 3][:], src, src[row0:row0 + 128, :])

        NT = len(tiles)
        load(0)
        if NT > 1:
            load(1)
        stage_a(0)
        for n in range(NT):
            if n + 2 < NT:
                load(n + 2)
            if n + 1 < NT:
                stage_a(n + 1)
            stage_b(n)
        P.end()

    def phase_P(self, layer, tiles):
        P = self.P
        P.begin()
        wb = [P.sb([128, 8, 512], BF16, "win%d" % i) for i in range(3)]
        rows = P.sb([128, 2432], F32, "rowsP")
        oml = P.sb([128, 1024], F32, "oml")
        cs_all = {}
        for nm, src in (("own", self.cs_own), ("par", self.cs_par)):
            cs_all[nm] = P.sb([128, 16, 256], F32, "cs_" + nm)
            P.dma(P.sp, cs_all[nm], cs_all[nm][:], src, src[:].rearrange("(n p) c -> p n c", p=128))
        sq = P.sb([128, 512], F32, "sq")
        ss = P.sb([128, 8], F32, "ssP")
        qn = P.sb([128, 512], F32, "qn")
        t1 = P.sb([128, 512], F32, "t1")
        t2 = P.sb([128, 512], F32, "t2")
        qr = [P.sb([128, 512], BF16, "qr%d" % i) for i in range(2)]
        stT = [P.sb([128, 512], BF16, "stT%d" % i) for i in range(2)]
        stb = [P.sb([128, 512], BF16, "stb%d" % i) for i in range(3)]
        stf = [P.sb([128, 512], F32, "stf%d" % i) for i in range(4)]
        kkfs = [P.sb([128, 512], F32, "kkf%d" % i) for i in range(8)]
        P.dma(P.sp, rows, rows[:], self.rowv, self.rowv[layer][:, 4096:6528])
        if layer == 1:
            P.op(P.dve, lambda e: e.tensor_tensor(out=oml[:], in0=rows[:, 384:1408], in1=rows[:, 1408:2432],
                                                  op=ALU.subtract), [rows], [oml])
            P.op(P.act, lambda e: e.activation(out=oml[:], in_=oml[:], func=AF.Sigmoid), [oml], [oml])
        blocks = list(range(17))
        cnt = {"mm": 0, "tr": 0, "b": 0, "f": 0, "T": 0, "q": 0, "cs": 0, "kk": 0}

        def nxt(k, lst):
            v = lst[cnt[k] % len(lst)]
            cnt[k] += 1
            return v

        sq2 = [sq, P.sb([128, 512], F32, "sqb")]
        ss2 = [ss, P.sb([128, 8], F32, "ssPb")]
        qn2 = [qn, P.sb([128, 512], F32, "qnb")]

        def qk_block(typ, sub, w, items):
            nh = 4 if typ == "q" else 2
            wd_ = nh * 128
            gain = rows[:, 0:128] if typ == "q" else rows[:, 128:256]
            v3 = lambda ap: ap[:, :wd_].rearrange("p (h d) -> p h d", h=nh)
            gb = gain.unsqueeze(1).to_broadcast([128, nh, 128])
            ctx = {}

            def mm(i):
                srow, cs_src, cs_row = items[i]
                pb = nxt("mm", self.bank[0:4])
                for kc in range(8):
                    P.op(P.pe, lambda e: e.matmul(pb[:], self.hT[:, kc, srow:srow + 128], w[:, kc, :],
                                                  start=(kc == 0), stop=(kc == 7)), [self.hT, w], [pb], inc=(kc == 7))
                cst = None
                if cs_src is not None:
                    cbuf = cs_all["own" if cs_src is self.cs_own else "par"]
                    cst = _View(cbuf, cbuf[:, cs_row // 128, :])
                ctx[i] = dict(pb=pb, cst=cst, q_out=qr[i % 2])

            def s1a(i):
                pb = ctx[i]["pb"]
                sq_, ss_ = sq2[i % 2], ss2[i % 2]
                P.op(P.act, lambda e: e.activation(out=sq_[:, :wd_], in_=pb[:, :wd_], func=AF.Square), [pb], [sq_])
                P.op(P.dve, lambda e: e.tensor_reduce(out=ss_[:, 0:nh], in_=sq_[:, :wd_].rearrange("p (h d) -> p h d", h=nh),
                                                      axis=AX.X, op=ALU.add), [sq_], [ss_])
                P.op(P.act, lambda e: e.activation(out=ss_[:, 4:4 + nh], in_=ss_[:, 0:nh], func=AF.Sqrt, scale=1.0 / 128, bias=EPS),
                     [ss_], [ss_])

            def s1b(i):
                pb, cst, q_out = ctx[i]["pb"], ctx[i]["cst"], ctx[i]["q_out"]
                ss_, qn_ = ss2[i % 2], qn2[i % 2]
                P.op(P.dve, lambda e: e.reciprocal(out=ss_[:, 0:nh], in_=ss_[:, 4:4 + nh]), [ss_], [ss_])
                P.op(P.dve, lambda e: e.tensor_tensor(out=v3(qn_), in0=pb[:, :wd_].rearrange("p (h d) -> p h d", h=nh),
                                                      in1=ss_[:, 0:nh].unsqueeze(2).to_broadcast([128, nh, 128]), op=ALU.mult),
                     [pb, ss_], [qn_])
                if cst is not None:
                    P.op(P.dve, lambda e: e.tensor_tensor(out=v3(qn_), in0=v3(qn_), in1=gb, op=ALU.mult), [qn_, rows], [qn_])
                else:
                    P.op(P.dve, lambda e: e.tensor_tensor(out=v3(q_out), in0=v3(qn_), in1=gb, op=ALU.mult), [qn_, rows], [q_out])

            def s1c(i):
                cst, q_out = ctx[i]["cst"], ctx[i]["q_out"]
                qn_ = qn2[i % 2]
                if cst is None:
                    return
                P.op(P.dve, lambda e: e.tensor_tensor(out=v3(t2)[:, :, 0:64], in0=v3(qn_)[:, :, 64:128],
                                                       in1=cst[:, 128:192].unsqueeze(1).to_broadcast([128, nh, 64]), op=ALU.mult),
                     [qn_, cst.buf], [t2])
                P.op(P.dve, lambda e: e.tensor_tensor(out=v3(t2)[:, :, 64:128], in0=v3(qn_)[:, :, 0:64],
                                                       in1=cst[:, 192:256].unsqueeze(1).to_broadcast([128, nh, 64]), op=ALU.mult),
                     [qn_, cst.buf, t2], [t2])
                P.op(P.dve, lambda e: e.tensor_tensor(out=v3(t1), in0=v3(qn_),
                                                      in1=cst[:, 0:128].unsqueeze(1).to_broadcast([128, nh, 128]), op=ALU.mult),
                     [qn_, cst.buf], [t1])
                P.op(P.dve, lambda e: e.tensor_tensor(out=q_out[:, :wd_], in0=t1[:, :wd_], in1=t2[:, :wd_], op=ALU.add),
                     [t1, t2], [q_out])

            def s2(i):
                srow = items[i][0]
                pb, q_out = ctx[i]["pb"], ctx[i]["q_out"]
                pt = nxt("tr", [self.bank[4], self.bank[5]])
                ptb = pt[:].bitcast(BF16)
                for h in range(nh):
                    P.op(P.pe, lambda e: e.transpose(out=ptb[:, h * 128:(h + 1) * 128], in_=q_out[:, h * 128:(h + 1) * 128],
                                                     identity=self.ident_bf[:]), [q_out, self.ident_bf], [pt], inc=(h == nh - 1))
                st = nxt("T", stT)
                P.op(P.act, lambda e: e.copy(out=st[:, :wd_], in_=ptb[:, :wd_]), [pt], [st])
                if typ == "q":
                    dbuf, dap = self.QT, self.QT[sub * 4:sub * 4 + 4, :, srow:srow + 128].rearrange("h d t -> d h t")
                else:
                    dbuf, dap = self.KT, self.KT[:, :, srow:srow + 128].rearrange("h d t -> d h t")
                P.dma(P.sp, dbuf, dap, st, st[:, :wd_].rearrange("p (h t) -> p h t", h=nh))
                if typ == "kv":
                    sv = nxt("b", stb)
                    P.op(P.act, lambda e: e.copy(out=sv[:, :256], in_=pb[:, 256:512]), [pb], [sv])
                    P.dma(P.sp, self.V, self.V[srow:srow + 128, :], sv, sv[:, :256])
                del ctx[i]

            N = len(items)
            mm(0)
            s1a(0)
            s1b(0)
            for i in range(N):
                if i + 1 < N:
                    mm(i + 1)
                    s1a(i + 1)
                s1c(i)
                s2(i)
                if i + 1 < N:
                    s1b(i + 1)

        def simple(psv, func, scale, dst_buf, dst_ap):
            st = nxt("b", stb)
            if func is None:
                P.op(P.act, lambda e: e.copy(out=st[:, :psv.shape[1]], in_=psv), [psb], [st])
            else:
                P.op(P.act, lambda e: e.activation(out=st[:, :psv.shape[1]], in_=psv, func=func, scale=scale), [psb], [st])
            P.dma(P.sp, dst_buf, dst_ap, st, st[:, :psv.shape[1]])

        pending = []

        def flush_ln():
            for (kkf_, ld_, rs_, c0_) in pending:
                sf = nxt("f", stf)
                P.op(P.act, lambda e: e.activation(out=sf[:], in_=kkf_[:], func=AF.Ln, scale=-1.0, bias=1.0), [kkf_], [sf])
                P.dma(P.sp, ld_, ld_[rs_, c0_:c0_ + 512], sf, sf[:])
            del pending[:]

        for blk in blocks:
            typ = BLK_TYPES[blk]
            w = nxt("b", [wb[0]]) if False else wb[blocks.index(blk) % 3]
            src = self.w_in[layer][:, blk * 512:(blk + 1) * 512].rearrange("(kc k) j -> k kc j", k=128)
            P.dma(P.pool, w, w[:], self.w_in, src)
            sub = blk % 2 if blk != 2 else 0
            if blk <= 1:
                sub = blk
            elif blk >= 3:
                sub = (blk - 3) % 2
            if typ in ("q", "kv"):
                qk_block(typ, sub, w, [(t_[0], t_[1], t_[2]) for t_ in tiles if t_[3] is None or blk in t_[3]])
                continue
            for (srow, cs_src, cs_row, tblocks) in tiles:
                if tblocks is not None and blk not in tblocks:
                    continue
                hcol = srow
                is_lat = cs_src is not None
                psb = nxt("mm", self.bank[0:4])
                for kc in range(8):
                    P.op(P.pe, lambda e: e.matmul(psb[:], self.hT[:, kc, hcol:hcol + 128], w[:, kc, :],
                                                  start=(kc == 0), stop=(kc == 7)), [self.hT, w], [psb], inc=(kc == 7))
                c0 = sub * 512
                rs = slice(srow, srow + 128)
                if False:
                    pass
                elif typ == "hq":
                    simple(psb[:], AF.Silu, 1.0, self.HQ, self.HQ[rs, c0:c0 + 512])
                elif typ == "hg":
                    simple(psb[:], AF.Silu, 1.0, self.HG, self.HG[rs, c0:c0 + 512])
                elif typ == "ga":
                    simple(psb[:], AF.Sigmoid, 1.0, self.GA, self.GA[rs, c0:c0 + 512])
                elif typ == "gh":
                    simple(psb[:], AF.Sigmoid, 1.0, self.GH, self.GH[rs, c0:c0 + 512])
                elif typ == "hi":
                    simple(psb[:], None, None, self.HI, self.HI[rs, c0:c0 + 512])
                else:
                    kd, ld = (self.KA, self.LFA) if typ == "fA" else (self.KB, self.LFB)
                    kkf = nxt("kk", kkfs)
                    P.op(P.act, lambda e: e.activation(out=kkf[:], in_=psb[:], func=AF.Sigmoid, scale=-1.0), [psb], [kkf])
                    if layer == 1:
                        P.op(P.dve, lambda e: e.tensor_tensor(out=kkf[:], in0=kkf[:], in1=oml[:, c0:c0 + 512], op=ALU.mult),
                             [kkf, oml], [kkf])
                    st = nxt("b", stb)
                    P.op(P.pool, lambda e: e.tensor_copy(out=st[:], in_=kkf[:]), [kkf], [st])
                    P.dma(P.sp, kd, kd[rs, c0:c0 + 512], st, st[:])
                    pending.append((kkf, ld, rs, c0))
                    if len(pending) == 4:
                        flush_ln()
            flush_ln()
        P.end()

    def phase_A(self, layer, groups):
        P = self.P
        P.begin()
        KTall = P.sb([128, 2, NKEY], BF16, "KTall")
        Vall = P.sb([128, 34, 256], BF16, "Vall")
        qt = [P.sb([128, 8, 512], BF16, "qtA%d" % i) for i in range(2)]
        pT = [P.sb([128, 512], BF16, "pT%d" % i) for i in range(4)]
        rden = P.sb([128, 512], F32, "rden")
        ost = [P.sb([128, 512], BF16, "ost%d" % i) for i in range(2)]
        for g in range(2):
            P.dma(P.sp, KTall, KTall[:, g, :], self.KT, self.KT[g])
        P.dma(P.sp, Vall, Vall[:], self.V, self.V[:].rearrange("(kb p) c -> p kb c", p=128))
        scale = 1.0 / float(np.sqrt(128.0))
        cnt = 0
        for gidx, (col0, n, kbs) in enumerate(groups):
            q = qt[gidx % 2]
            P.dma(P.sp, q, q[:, :, :n], self.QT, self.QT[:, :, col0:col0 + n].rearrange("h d t -> d h t"))
            for h in range(8):
                g = h // 4
                pso = self.bank[2 + (cnt % 2)]
                psd = self.bank[4 + (cnt % 2)]
                o_st = ost[cnt % 2]
                cnt += 1

                sbanks = [self.bank[0], self.bank[1], self.bank[6]]

                def S(i):
                    kb = kbs[i]
                    ps = sbanks[i % 3]
                    P.op(P.pe, lambda e: e.matmul(ps[:, :n], KTall[:, g, kb * 128:(kb + 1) * 128], q[:, h, :n],
                                                  start=True, stop=True), [KTall, q], [ps])
                S(0)
                if len(kbs) > 1:
                    S(1)
                for i, kb in enumerate(kbs):
                    if i + 2 < len(kbs):
                        S(i + 2)
                    ps = sbanks[i % 3]
                    p = pT[i % 4]
                    P.op(P.act, lambda e: e.activation(out=p[:, :n], in_=ps[:, :n], func=AF.Exp, scale=scale), [ps], [p])
                    first, lst = (i == 0), (i == len(kbs) - 1)
                    P.op(P.pe, lambda e: e.matmul(pso[:, :n], Vall[:, kb, g * 128:(g + 1) * 128], p[:, :n],
                                                  start=first, stop=lst), [Vall, p], [pso], inc=False)
                    P.op(P.pe, lambda e: e.matmul(psd[:, :n], self.ones_bf[:], p[:, :n],
                                                  start=first, stop=lst), [self.ones_bf, p], [psd])
                P.op(P.dve, lambda e: e.reciprocal(out=rden[:, :n], in_=psd[:, :n]), [psd], [rden])
                P.op(P.dve, lambda e: e.tensor_tensor(out=o_st[:, :n], in0=pso[:, :n], in1=rden[:, :n], op=ALU.mult),
                     [pso, rden], [o_st])
                P.dma(P.sp, self.ATT, self.ATT[h, :, col0:col0 + n], o_st, o_st[:, :n])
        P.end()

    def phase_G(self, layer, last, full):
        P = self.P
        P.begin()
        c = self.c_f32
        M = {0: (c[:, 128:256], c[:, 256:384], c[:, 384:512]), 1: (c[:, 512:640], c[:, 640:768], c[:, 768:896])}
        ind = c[:, 896:898]
        mask = [P.sb([128, 128], U32, "mask%d" % d) for d in range(2)]
        for d in range(2):
            P.op(P.dve, lambda e: e.tensor_scalar(out=mask[d][:], in0=M[d][2], scalar1=0.5, scalar2=None, op0=ALU.is_gt),
                 [c], [mask[d]])
        S = [P.sb([128, 128], F32, "S%d" % h) for h in range(8)]
        Sb = [P.sb([128, 128], BF16, "Sb%d" % h) for h in range(8)]
        Am = [[P.sb([128, 4, 128], BF16, "Am%d%d" % (d, i)) for i in range(2)] for d in range(2)]
        qhf = [[P.sb([128, 4, 128], BF16, "qhf%d%d" % (d, i)) for i in range(2)] for d in range(2)]
        qhs = [[P.sb([128, 4, 128], BF16, "qhs%d%d" % (d, i)) for i in range(2)] for d in range(2)]
        for grp in (Am, qhf, qhs):
            for d in range(2):
                for b_ in grp[d]:
                    P.op(P.dve, lambda e: e.memset(b_[:], 0.0), [], [b_])
        ld = [dict(kk=P.sb([128, D], BF16, "kk%d" % i), lf=P.sb([128, D], F32, "lf%d" % i),
                   hi=P.sb([128, D], BF16, "hi%d" % i), hq=P.sb([128, D], BF16, "hq%d" % i)) for i in range(2)]
        e1 = P.sb([128, 512], F32, "e1"); e1n = P.sb([128, 512], F32, "e1n")
        e3 = P.sb([128, 512], F32, "e3"); e2 = P.sb([128, 512], F32, "e2")
        dec = [P.sb([128, 8], F32, "dec%d" % i) for i in range(2)]
        qt = P.sb([128, 512], BF16, "qt"); kt = P.sb([128, 512], BF16, "kt")
        qh = P.sb([128, 512], BF16, "qh")
        kh = [P.sb([128, 512], BF16, "kh%d" % i) for i in range(2)]
        qtT = P.sb([128, 512], BF16, "qtT"); ktT = P.sb([128, 512], BF16, "ktT")
        ostage = [P.sb([128, D], F32, "ostage%d" % i) for i in range(2)]
        bk = self.bank

        def reset_state():
            for h in range(8):
                P.op(P.dve, lambda e: e.memset(S[h][:], 0.0), [], [S[h]])
                P.op(P.dve, lambda e: e.memset(Sb[h][:], 0.0), [], [Sb[h]])

        plan = []

        def tile(d, row0, srcK, srcLF, full, outd):
            plan.append(("tile", d, row0, srcK, srcLF, full, outd))

        def tile_loads(idx, d, row0, srcK, srcLF, full, outd):
            L = ld[idx % 2]
            rs = slice(row0, row0 + 128)
            P.dma(P.sp, L["kk"], L["kk"][:], srcK, srcK[rs, :])
            P.dma(P.sp, L["lf"], L["lf"][:], srcLF, srcLF[rs, :])
            P.dma(P.sp, L["hi"], L["hi"][:], self.HI, self.HI[rs, :])
            if full:
                P.dma(P.sp, L["hq"], L["hq"][:], self.HQ, self.HQ[rs, :])

        def front(u, idx, half, d, row0, srcK, srcLF, full, outd):
            L = ld[idx % 2]
            kk, lf, hi, hq = L["kk"], L["lf"], L["hi"], L["hq"]
            fc, sc = (0, 1) if d == 0 else (1, 0)
            fr = slice(fc * 64, fc * 64 + 64)
            sr = slice(sc * 64, sc * 64 + 64)
            M1, M2, M3 = M[d]
            pu = u % 2
            c0 = half * 512
            cs_ = slice(c0, c0 + 512)
            P.op(P.pe, lambda e: e.matmul(bk[1][:], M2, lf[:, cs_], start=True, stop=True), [c, lf], [bk[1]])
            psD = bk[4][:, 256:264]
            for hh in range(4):
                P.op(P.pe, lambda e: e.matmul(psD[:, hh * 2:hh * 2 + 2], lf[:, c0 + hh * 128:c0 + (hh + 1) * 128], ind,
                                              start=True, stop=True), [c, lf], [bk[4]], inc=(hh == 3))
            if full:
                P.op(P.pe, lambda e: e.matmul(bk[0][:], M1, lf[:, cs_], start=True, stop=True), [c, lf], [bk[0]])
            P.op(P.act, lambda e: e.activation(out=e2[:], in_=bk[1][:], func=AF.Exp), [bk[1]], [e2])
            P.op(P.act, lambda e: e.activation(out=dec[pu][:], in_=psD, func=AF.Exp), [bk[4]], [dec[pu]])
            if full:
                P.op(P.pe, lambda e: e.matmul(bk[1][:], M3, lf[:, cs_], start=True, stop=True), [c, lf], [bk[1]])
            P.op(P.dve, lambda e: e.tensor_tensor(out=kh[pu][:], in0=kk[:, cs_], in1=e2[:], op=ALU.mult), [kk, e2], [kh[pu]])
            if not full:
                return
            P.op(P.act, lambda e: e.activation(out=e1[:], in_=bk[0][:], func=AF.Exp), [bk[0]], [e1])
            P.op(P.act, lambda e: e.activation(out=e1n[:], in_=bk[0][:], func=AF.Exp, scale=-1.0), [bk[0]], [e1n])
            P.op(P.act, lambda e: e.activation(out=e3[:], in_=bk[1][:], func=AF.Exp), [bk[1]], [e3])
            P.op(P.dve, lambda e: e.tensor_tensor(out=qt[:], in0=hq[:, cs_], in1=e1[:], op=ALU.mult), [hq, e1], [qt])
            P.op(P.dve, lambda e: e.tensor_tensor(out=kt[:], in0=kk[:, cs_], in1=e1n[:], op=ALU.mult), [kk, e1n], [kt])
            P.op(P.dve, lambda e: e.tensor_tensor(out=qh[:], in0=hq[:, cs_], in1=e3[:], op=ALU.mult), [hq, e3], [qh])
            T1 = bk[3][:].bitcast(BF16)
            T2 = bk[4][:].bitcast(BF16)
            for hh in range(4):
                hs = slice(hh * 128, (hh + 1) * 128)
                P.op(P.pe, lambda e: e.transpose(out=T1[:, hs], in_=qt[:, hs], identity=self.ident_bf[:]),
                     [qt, self.ident_bf], [bk[3]], inc=False)
            for hh in range(4):
                hs = slice(hh * 128, (hh + 1) * 128)
                P.op(P.pe, lambda e: e.transpose(out=T1[:, 512 + hh * 128:512 + (hh + 1) * 128], in_=kt[:, hs],
                                                 identity=self.ident_bf[:]), [kt, self.ident_bf], [bk[3]], inc=(hh == 3))
            for hh in range(4):
                hs = slice(hh * 128, (hh + 1) * 128)
                P.op(P.pe, lambda e: e.transpose(out=T2[:, hs], in_=qh[:, hs], identity=self.ident_bf[:]),
                     [qh, self.ident_bf], [bk[4]], inc=(hh == 3))
            P.op(P.act, lambda e: e.copy(out=qtT[:], in_=T1[:, 0:512]), [bk[3]], [qtT])
            P.op(P.act, lambda e: e.copy(out=ktT[:], in_=T1[:, 512:1024]), [bk[3]], [ktT])
            T2v = T2[:, 0:512].rearrange("p (h t) -> p h t", h=4)
            qf, qs, am = qhf[d][pu], qhs[d][pu], Am[d][pu]
            P.op(P.dve, lambda e: e.tensor_copy(out=qf[:, :, fr], in_=T2v[:, :, fr]), [bk[4]], [qf])
            P.op(P.dve, lambda e: e.tensor_copy(out=qs[:, :, sr], in_=T2v[:, :, sr]), [bk[4]], [qs])
            for hh in range(4):
                hs = slice(hh * 128, (hh + 1) * 128)
                P.op(P.pe, lambda e: e.matmul(bk[5][:, hs], ktT[:, hs], qtT[:, hs], start=True, stop=True),
                     [ktT, qtT], [bk[5]], inc=(hh == 3))
            P.op(P.dve, lambda e: e.copy_predicated(
                out=am[:], mask=mask[d][:].unsqueeze(1).to_broadcast([128, 4, 128]),
                data=bk[5][:].rearrange("p (h t) -> p h t", h=4)), [bk[5], mask[d]], [am])

        def back(u, idx, half, d, row0, srcK, srcLF, full, outd):
            L = ld[idx % 2]
            hi = L["hi"]
            ost = ostage[idx % 2]
            fc, sc = (0, 1) if d == 0 else (1, 0)
            fr = slice(fc * 64, fc * 64 + 64)
            sr = slice(sc * 64, sc * 64 + 64)
            pu = u % 2
            c0 = half * 512
            k_h, de = kh[pu], dec[pu]
            qf, qs, am = qhf[d][pu], qhs[d][pu], Am[d][pu]
            HS = [slice(hh * 128, (hh + 1) * 128) for hh in range(4)]
            HC = [slice(c0 + hh * 128, c0 + (hh + 1) * 128) for hh in range(4)]
            for hh in range(4):
                P.op(P.pe, lambda e: e.matmul(bk[7][:, HS[hh]], k_h[fr, HS[hh]], hi[fr, HC[hh]], start=True, stop=True),
                     [k_h, hi], [bk[7]], inc=False)
                P.op(P.pe, lambda e: e.matmul(bk[2][:, HS[hh]], k_h[sr, HS[hh]], hi[sr, HC[hh]], start=True, stop=True),
                     [k_h, hi], [bk[2]], inc=(hh == 3))
            if full:
                for hh in range(4):
                    h = half * 4 + hh
                    P.op(P.pe, lambda e: e.matmul(bk[6][:, HS[hh]], am[:, hh, :], hi[:, HC[hh]], start=True, stop=False),
                         [am, hi], [bk[6]], inc=False)
                    P.op(P.pe, lambda e: e.matmul(bk[6][:, HS[hh]], qf[:, hh, :], Sb[h][:], start=False, stop=True),
                         [qf, Sb[h]], [bk[6]])
            for hh in range(4):
                h = half * 4 + hh
                P.op(P.dve, lambda e: e.scalar_tensor_tensor(out=S[h][:], in0=S[h][:], scalar=de[:, hh * 2 + fc:hh * 2 + fc + 1],
                                                             in1=bk[7][:, HS[hh]], op0=ALU.mult, op1=ALU.add),
                     [S[h], de, bk[7]], [S[h]])
            for hh in range(4):
                h = half * 4 + hh
                P.op(P.act, lambda e: e.copy(out=Sb[h][:], in_=S[h][:]), [S[h]], [Sb[h]])
            if full:
                for hh in range(4):
                    h = half * 4 + hh
                    P.op(P.pe, lambda e: e.matmul(bk[0][:, HS[hh]], qs[:, hh, :], Sb[h][:], start=True, stop=True),
                         [qs, Sb[h]], [bk[0]])
            for hh in range(4):
                h = half * 4 + hh
                P.op(P.dve, lambda e: e.scalar_tensor_tensor(out=S[h][:], in0=S[h][:], scalar=de[:, hh * 2 + sc:hh * 2 + sc + 1],
                                                             in1=bk[2][:, HS[hh]], op0=ALU.mult, op1=ALU.add),
                     [S[h], de, bk[2]], [S[h]])
            for hh in range(4):
                h = half * 4 + hh
                P.op(P.act, lambda e: e.copy(out=Sb[h][:], in_=S[h][:]), [S[h]], [Sb[h]])
            if full:
                P.op(P.act, lambda e: e.copy(out=ost[:, c0:c0 + 512], in_=bk[6][:]), [bk[6]], [ost])
                P.op(P.dve, lambda e: e.tensor_tensor(out=ost[:, c0:c0 + 512], in0=ost[:, c0:c0 + 512], in1=bk[0][:], op=ALU.add),
                     [ost, bk[0]], [ost])
                if half == 1:
                    P.dma(P.sp, outd, outd[row0:row0 + 128, :], ost, ost[:])

        ctx_full = not last
        plan.append(("reset",))
        for i in (16, 17):
            tile(0, i * 128, self.KA, self.LFA, ctx_full, self.OA)
        for i in range(16):
            tile(0, i * 128, self.KA, self.LFA, True, self.OA)
        if full:
            for j in range(15, -1, -1):
                tile(1, NTOK + j * 128, self.KA, self.LFA, True, self.OB)
        plan.append(("reset",))
        for i in (17, 16):
            tile(1, i * 128, self.KB, self.LFB, ctx_full, self.OB)
        for j in range(16):
            tile(0, NTOK + j * 128, self.KB, self.LFB, full, self.OA)
        for i in range(15, -1, -1):
            tile(1, i * 128, self.KB, self.LFB, True, self.OB)

        units = []
        idx = 0
        pending_reset = False
        for p in plan:
            if p[0] == "reset":
                pending_reset = True
                continue
            for half in range(2):
                units.append((pending_reset and half == 0, idx, half, p[1:]))
            pending_reset = False
            idx += 1
        tl = [p[1:] for p in plan if p[0] == "tile"]
        tile_loads(0, *tl[0])
        if len(tl) > 1:
            tile_loads(1, *tl[1])
        front(0, units[0][1], units[0][2], *units[0][3])
        for u, (rst, idx, half, args) in enumerate(units):
            if u + 1 < len(units):
                _, i2, h2, a2 = units[u + 1]
                front(u + 1, i2, h2, *a2)
            if rst:
                reset_state()
            back(u, idx, half, *args)
            if half == 1 and idx + 2 < len(tl):
                tile_loads(idx + 2, *tl[idx + 2])
        P.end()

    def resid(self, zb, xt, xidx, gi, xo, ss, tmp):
        P = self.P
        junk = tmp
        for hf in range(2):
            P.op(P.act, lambda e: e.activation(out=junk[:, 0:512], in_=zb[hf][1], func=AF.Square, accum_out=ss[:, hf:hf + 1]),
                 [zb[hf][0]], [junk, ss])
        P.op(P.dve, lambda e: e.tensor_tensor(out=ss[:, 2:3], in0=ss[:, 0:1], in1=ss[:, 1:2], op=ALU.add), [ss], [ss])
        P.op(P.act, lambda e: e.activation(out=ss[:, 3:4], in_=ss[:, 2:3], func=AF.Sqrt, scale=1.0 / D, bias=EPS), [ss], [ss])
        P.op(P.dve, lambda e: e.reciprocal(out=ss[:, 2:3], in_=ss[:, 3:4]), [ss], [ss])
        for hf in range(2):
            cs_ = slice(hf * 512, (hf + 1) * 512)
            P.op(P.dve, lambda e: e.tensor_scalar(out=tmp[:, cs_], in0=zb[hf][1], scalar1=ss[:, 2:3], scalar2=None, op0=ALU.mult),
                 [zb[hf][0], ss], [tmp])
            P.op(P.dve, lambda e: e.tensor_tensor(out=tmp[:, cs_], in0=tmp[:, cs_], in1=self.grow[:, xidx, gi, cs_], op=ALU.mult),
                 [tmp, self.grow], [tmp])
            P.op(P.dve, lambda e: e.tensor_tensor(out=xo[:, cs_], in0=tmp[:, cs_], in1=xt[:, cs_], op=ALU.add), [tmp, xt], [xo])

    def phase_X(self, layer, tiles, xsrc, xdst):
        P = self.P
        P.begin()
        W = {}
        for nm, src in (("att", self.w_att), ("hg", self.w_hg), ("out", self.w_out)):
            W[nm] = P.sb([128, 8, D], BF16, "W" + nm)
            P.dma(P.pool, W[nm], W[nm][:], src, src[layer].rearrange("(kc k) j -> k kc j", k=128))
        hgn = P.sb([128, 128], F32, "hgn")
        P.dma(P.sp, hgn, hgn[:], self.rowv, self.rowv[layer][:, 4352:4480])
        L = [dict(oa=P.sb([128, D], F32, "oa%d" % i), ob=P.sb([128, D], F32, "ob%d" % i),
                  hg=P.sb([128, D], BF16, "hg%d" % i), ga=P.sb([128, D], BF16, "ga%d" % i),
                  gh=P.sb([128, D], BF16, "gh%d" % i), at=P.sb([128, 8, 128], BF16, "at%d" % i),
                  x=P.sb([128, D], F32, "x%d" % i)) for i in range(3)]
        junk = P.sb([128, D], F32, "junkX")
        junk2 = P.sb([128, D], F32, "junkX2")
        ss = P.sb([128, 16], F32, "ssX")
        ss2 = P.sb([128, 4], F32, "ss2X")
        hgo = [P.sb([128, D], BF16, "hgo%d" % i) for i in range(2)]
        hgT = [P.sb([128, 8, 128], BF16, "hgT%d" % i) for i in range(2)]
        y1 = P.sb([128, 512], F32, "y1")
        y2 = P.sb([128, 512], F32, "y2")
        ybf = [P.sb([128, D], BF16, "ybf%d" % i) for i in range(2)]
        yT = [P.sb([128, 8, 128], BF16, "yT%d" % i) for i in range(2)]
        xo = [P.sb([128, D], F32, "xo%d" % i) for i in range(2)]
        bk = self.bank
        NT = len(tiles)

        def loads(it):
            n = tiles[it]
            l = L[it % 3]
            rs = slice(n * 128, (n + 1) * 128)
            P.dma(P.sp, l["oa"], l["oa"][:], self.OA, self.OA[rs, :])
            P.dma(P.sp, l["ob"], l["ob"][:], self.OB, self.OB[rs, :])
            P.dma(P.sp, l["hg"], l["hg"][:], self.HG, self.HG[rs, :])
            P.dma(P.sp, l["ga"], l["ga"][:], self.GA, self.GA[rs, :])
            P.dma(P.sp, l["gh"], l["gh"][:], self.GH, self.GH[rs, :])
            P.dma(P.sp, l["at"], l["at"][:], self.ATT, self.ATT[:, :, n * 128:(n + 1) * 128].rearrange("h d t -> d h t"))
            xb, xr = xsrc(n)
            P.dma(P.sp, l["x"], l["x"][:], xb, xb[xr:xr + 128, :])

        def readout(it):
            l = L[it % 3]
            o = l["oa"]
            hg_o = hgo[it % 2]
            P.op(P.pool, lambda e: e.tensor_tensor(out=o[:], in0=o[:], in1=l["ob"][:], op=ALU.add), [o, l["ob"]], [o])
            P.op(P.act, lambda e: e.activation(out=junk[:], in_=o[:], func=AF.Square), [o], [junk])
            P.op(P.dve, lambda e: e.tensor_reduce(out=ss[:, 0:8], in_=junk[:].rearrange("p (h d) -> p h d", h=8), axis=AX.X, op=ALU.add),
                 [junk], [ss])
            P.op(P.act, lambda e: e.activation(out=ss[:, 8:16], in_=ss[:, 0:8], func=AF.Sqrt, scale=1.0 / 128, bias=EPS), [ss], [ss])
            P.op(P.dve, lambda e: e.reciprocal(out=ss[:, 0:8], in_=ss[:, 8:16]), [ss], [ss])
            o3 = o[:].rearrange("p (h d) -> p h d", h=8)
            P.op(P.dve, lambda e: e.tensor_tensor(out=o3, in0=o3, in1=ss[:, 0:8].unsqueeze(2).to_broadcast([128, 8, 128]), op=ALU.mult),
                 [o, ss], [o])
            P.op(P.dve, lambda e: e.tensor_tensor(out=o3, in0=o3, in1=hgn[:].unsqueeze(1).to_broadcast([128, 8, 128]), op=ALU.mult),
                 [o, hgn], [o])
            P.op(P.pool, lambda e: e.tensor_tensor(out=hg_o[:], in0=o[:], in1=l["hg"][:], op=ALU.mult), [o, l["hg"]], [hg_o])

        def branch_mm(it):
            l = L[it % 3]
            hg_o = hgo[it % 2]
            hg_T = hgT[it % 2]
            T0 = bk[0][:].bitcast(BF16)
            for kc in range(8):
                P.op(P.pe, lambda e: e.transpose(out=T0[:, kc * 128:(kc + 1) * 128], in_=hg_o[:, kc * 128:(kc + 1) * 128],
                                                 identity=self.ident_bf[:]), [hg_o, self.ident_bf], [bk[0]], inc=(kc == 7))
            P.op(P.act, lambda e: e.copy(out=hg_T[:].rearrange("p k t -> p (k t)"), in_=T0[:, :]), [bk[0]], [hg_T])
            for hf in range(2):
                cs_ = slice(hf * 512, (hf + 1) * 512)
                for kc in range(8):
                    P.op(P.pe, lambda e: e.matmul(bk[2 + hf][:], l["at"][:, kc, :], W["att"][:, kc, cs_], start=(kc == 0), stop=(kc == 7)),
                         [l["at"], W["att"]], [bk[2 + hf]], inc=(kc == 7))
                for kc in range(8):
                    P.op(P.pe, lambda e: e.matmul(bk[4 + hf][:], hg_T[:, kc, :], W["hg"][:, kc, cs_], start=(kc == 0), stop=(kc == 7)),
                         [hg_T, W["hg"]], [bk[4 + hf]], inc=(kc == 7))

        def gate_merge(it):
            l = L[it % 3]
            y_bf = ybf[it % 2]
            for hf in range(2):
                cs_ = slice(hf * 512, (hf + 1) * 512)
                P.op(P.dve, lambda e: e.tensor_tensor(out=y1[:], in0=bk[2 + hf][:], in1=l["ga"][:, cs_], op=ALU.mult),
                     [bk[2 + hf], l["ga"]], [y1])
                P.op(P.dve, lambda e: e.tensor_tensor(out=y2[:], in0=bk[4 + hf][:], in1=l["gh"][:, cs_], op=ALU.mult),
                     [bk[4 + hf], l["gh"]], [y2])
                P.op(P.dve, lambda e: e.tensor_tensor(out=y_bf[:, cs_], in0=y1[:], in1=y2[:], op=ALU.add), [y1, y2], [y_bf])

        def out_mm(it):
            y_bf = ybf[it % 2]
            y_T = yT[it % 2]
            T1 = bk[1][:].bitcast(BF16)
            for kc in range(8):
                P.op(P.pe, lambda e: e.transpose(out=T1[:, kc * 128:(kc + 1) * 128], in_=y_bf[:, kc * 128:(kc + 1) * 128],
                                                 identity=self.ident_bf[:]), [y_bf, self.ident_bf], [bk[1]], inc=(kc == 7))
            P.op(P.act, lambda e: e.copy(out=y_T[:].rearrange("p k t -> p (k t)"), in_=T1[:, :]), [bk[1]], [y_T])
            for hf in range(2):
                cs_ = slice(hf * 512, (hf + 1) * 512)
                for kc in range(8):
                    P.op(P.pe, lambda e: e.matmul(bk[6 + hf][:], y_T[:, kc, :], W["out"][:, kc, cs_], start=(kc == 0), stop=(kc == 7)),
                         [y_T, W["out"]], [bk[6 + hf]], inc=(kc == 7))

        def finish_tile(it):
            n = tiles[it]
            l = L[it % 3]
            xidx = 1 if n in (16, 17) else 0
            x_o = xo[it % 2]
            self.resid([(bk[6], bk[6][:]), (bk[7], bk[7][:])], l["x"], xidx, 0, x_o, ss2, junk2)
            P.dma(P.sp, xdst, xdst[n * 128:(n + 1) * 128, :], x_o, x_o[:])

        loads(0)
        if NT > 1:
            loads(1)
        readout(0)
        branch_mm(0)
        gate_merge(0)
        for it in range(NT):
            out_mm(it)
            if it + 2 < NT:
                loads(it + 2)
            if it + 1 < NT:
                readout(it + 1)
                branch_mm(it + 1)
            finish_tile(it)
            if it + 1 < NT:
                gate_merge(it + 1)
        P.end()

    def phase_F(self, xsrc_buf, xdst_fn, tiles, groups):
        P = self.P
        P.begin()
        wg = [P.sb([128, 8, 512], BF16, "wg%d" % i) for i in range(2)]
        wu = [P.sb([128, 8, 512], BF16, "wu%d" % i) for i in range(2)]
        sg = [P.sb([128, 512], F32, "sg%d" % i) for i in range(2)]
        ast = [P.sb([128, 512], BF16, "ast%d" % i) for i in range(3)]
        bk = self.bank
        cnt = 0
        nblk = (DFF + 511) // 512
        for fb in range(nblk):
            wdt = min(512, DFF - fb * 512)
            g_, u_ = wg[fb % 2], wu[fb % 2]
            P.dma(P.pool, g_, g_[:, :, :wdt], self.ffn_g, self.ffn_g[:, fb * 512:fb * 512 + wdt].rearrange("(kc k) j -> k kc j", k=128))
            P.dma(P.pool, u_, u_[:, :, :wdt], self.ffn_u, self.ffn_u[:, fb * 512:fb * 512 + wdt].rearrange("(kc k) j -> k kc j", k=128))
            for fcl in range(wdt // 128):
                fc = fb * 4 + fcl
                fs = slice(fcl * 128, (fcl + 1) * 128)
                for (col0, n) in groups:
                    pg = bk[cnt % 2]
                    pu = bk[2 + cnt % 2]
                    s_ = sg[cnt % 2]
                    a_ = ast[cnt % 3]
                    cnt += 1
                    for kc in range(8):
                        P.op(P.pe, lambda e: e.matmul(pg[:, :n], g_[:, kc, fs], self.hT[:, kc, col0:col0 + n], start=(kc == 0), stop=(kc == 7)),
                             [g_, self.hT], [pg], inc=(kc == 7))
                    for kc in range(8):
                        P.op(P.pe, lambda e: e.matmul(pu[:, :n], u_[:, kc, fs], self.hT[:, kc, col0:col0 + n], start=(kc == 0), stop=(kc == 7)),
                             [u_, self.hT], [pu], inc=(kc == 7))
                    P.op(P.act, lambda e: e.activation(out=s_[:, :n], in_=pg[:, :n], func=AF.Silu), [pg], [s_])
                    P.op(P.dve, lambda e: e.tensor_tensor(out=a_[:, :n], in0=s_[:, :n], in1=pu[:, :n], op=ALU.mult), [s_, pu], [a_])
                    P.dma(P.sp, self.AT, self.AT[fc, :, col0:col0 + n], a_, a_[:, :n])
        P.end()
        P.begin()
        NFC = DFF // 128
        wd = P.sb([128, NFC, D], BF16, "wd")
        P.dma(P.pool, wd, wd[:, 0:11, :], self.ffn_d, self.ffn_d[0:11 * 128, :].rearrange("(c f) j -> f c j", f=128))
        P.dma(P.pool, wd, wd[:, 11:22, :], self.ffn_d, self.ffn_d[11 * 128:22 * 128, :].rearrange("(c f) j -> f c j", f=128))
        aT = [P.sb([128, NFC, 128], BF16, "aT%d" % i) for i in range(2)]
        xt = [P.sb([128, D], F32, "xF%d" % i) for i in range(2)]
        xo = [P.sb([128, D], F32, "xoF%d" % i) for i in range(2)]
        tmp = P.sb([128, D], F32, "tmpF")
        ss = P.sb([128, 4], F32, "ssF")
        def loadsF(it):
            n = tiles[it]
            P.dma(P.sp, aT[it % 2], aT[it % 2][:], self.AT, self.AT[0:NFC, :, n * 128:(n + 1) * 128].rearrange("c f t -> f c t"))
            P.dma(P.sp, xt[it % 2], xt[it % 2][:], xsrc_buf, xsrc_buf[n * 128:(n + 1) * 128, :])

        loadsF(0)
        for it, n in enumerate(tiles):
            a_ = aT[it % 2]
            x_ = xt[it % 2]
            xidx = 1 if n in (16, 17) else 0
            rs = slice(n * 128, (n + 1) * 128)
            zb = [bk[4 + 2 * (it % 2)], bk[5 + 2 * (it % 2)]]
            for hf in range(2):
                for fc in range(NFC):
                    P.op(P.pe, lambda e: e.matmul(zb[hf][:], a_[:, fc, :], wd[:, fc, hf * 512:(hf + 1) * 512], start=(fc == 0), stop=(fc == NFC - 1)),
                         [a_, wd], [zb[hf]], inc=(fc == NFC - 1))
            if it + 1 < len(tiles):
                loadsF(it + 1)
            x_o = xo[it % 2]
            self.resid([(zb[0], zb[0][:]), (zb[1], zb[1][:])], x_, xidx, 1, x_o, ss, tmp)
            db, dr = xdst_fn(n)
            P.dma(P.sp, db, db[dr:dr + 128, :], x_o, x_o[:])
        P.end()

    def phase_R(self, xsrc_buf, gates):
        P = self.P
        P.begin()
        wr = P.sb([128, 8, NEXP], F32, "wr")
        P.dma(P.sp, wr, wr[:], self.moe_r, self.moe_r[:].rearrange("(kc k) e -> k kc e", k=128))
        xt = [P.sb([128, D], F32, "xR%d" % i) for i in range(2)]
        xs = P.sb([128, D], F32, "xsR")
        junk = P.sb([128, D], BF16, "junkR")
        fT = P.sb([128, 8, 128], F32, "fT32")
        ss = P.sb([128, 2], F32, "ssR")
        lg = P.sb([128, 8], F32, "lg")
        l2 = P.sb([128, 8], F32, "l2")
        eq1 = P.sb([128, 8], F32, "eq1")
        eq2 = P.sb([128, 8], F32, "eq2")
        sm = P.sb([128, 8], F32, "sm")
        bk = self.bank
        for n in range(16):
            a = xt[n % 2]
            P.dma(P.sp, a, a[:], xsrc_buf, xsrc_buf[n * 128:(n + 1) * 128, :])
            P.op(P.act, lambda e: e.activation(out=junk[:], in_=a[:], func=AF.Square, accum_out=ss[:, 0:1]), [a], [junk, ss])
            P.op(P.act, lambda e: e.activation(out=ss[:, 1:2], in_=ss[:, 0:1], func=AF.Sqrt, scale=1.0 / D, bias=EPS), [ss], [ss])
            P.op(P.dve, lambda e: e.reciprocal(out=ss[:, 0:1], in_=ss[:, 1:2]), [ss], [ss])
            P.op(P.dve, lambda e: e.tensor_scalar(out=xs[:], in0=a[:], scalar1=ss[:, 0:1], scalar2=None, op0=ALU.mult), [a, ss], [xs])
            for kc in range(8):
                pb = bk[kc // 4]
                P.op(P.pe, lambda e: e.transpose(out=pb[:, (kc % 4) * 128:(kc % 4 + 1) * 128], in_=xs[:, kc * 128:(kc + 1) * 128],
                                                 identity=self.ident_f), [xs, self.c_f32], [pb], inc=(kc % 4 == 3))
            for kc in range(8):
                pb = bk[kc // 4]
                P.op(P.dve, lambda e: e.tensor_scalar(out=fT[:, kc, :], in0=pb[:, (kc % 4) * 128:(kc % 4 + 1) * 128],
                                                      scalar1=self.AB[:, 2, kc, 0:1], scalar2=self.AB[:, 3, kc, 0:1],
                                                      op0=ALU.mult, op1=ALU.add), [pb, self.AB], [fT])
            for kc in range(8):
                P.op(P.pe, lambda e: e.matmul(bk[2][:, 0:NEXP], fT[:, kc, :], wr[:, kc, :], start=(kc == 0), stop=(kc == 7)),
                     [fT, wr], [bk[2]], inc=(kc == 7))
            P.op(P.dve, lambda e: e.tensor_copy(out=lg[:], in_=bk[2][:, 0:NEXP]), [bk[2]], [lg])
            P.op(P.dve, lambda e: e.tensor_reduce(out=sm[:, 0:1], in_=lg[:], axis=AX.X, op=ALU.max), [lg], [sm])
            P.op(P.dve, lambda e: e.tensor_scalar(out=eq1[:], in0=lg[:], scalar1=sm[:, 0:1], scalar2=None, op0=ALU.is_equal), [lg, sm], [eq1])
            P.op(P.dve, lambda e: e.scalar_tensor_tensor(out=l2[:], in0=eq1[:], scalar=-1e30, in1=lg[:], op0=ALU.mult, op1=ALU.add),
                 [eq1, lg], [l2])
            P.op(P.dve, lambda e: e.tensor_reduce(out=sm[:, 1:2], in_=l2[:], axis=AX.X, op=ALU.max), [l2], [sm])
            P.op(P.dve, lambda e: e.tensor_scalar(out=eq2[:], in0=l2[:], scalar1=sm[:, 1:2], scalar2=None, op0=ALU.is_equal), [l2, sm], [eq2])
            P.op(P.dve, lambda e: e.tensor_tensor(out=sm[:, 2:3], in0=sm[:, 1:2], in1=sm[:, 0:1], op=ALU.subtract), [sm], [sm])
            P.op(P.act, lambda e: e.activation(out=sm[:, 3:4], in_=sm[:, 2:3], func=AF.Exp), [sm], [sm])
            P.op(P.dve, lambda e: e.tensor_scalar(out=sm[:, 4:5], in0=sm[:, 3:4], scalar1=1.0, scalar2=None, op0=ALU.add), [sm], [sm])
            P.op(P.dve, lambda e: e.reciprocal(out=sm[:, 5:6], in_=sm[:, 4:5]), [sm], [sm])
            P.op(P.dve, lambda e: e.tensor_tensor(out=sm[:, 6:7], in0=sm[:, 3:4], in1=sm[:, 5:6], op=ALU.mult), [sm], [sm])
            P.op(P.dve, lambda e: e.tensor_scalar(out=eq1[:], in0=eq1[:], scalar1=sm[:, 5:6], scalar2=None, op0=ALU.mult), [eq1, sm], [eq1])
            P.op(P.dve, lambda e: e.scalar_tensor_tensor(out=gates[:, n, :], in0=eq2[:], scalar=sm[:, 6:7], in1=eq1[:], op0=ALU.mult, op1=ALU.add),
                 [eq2, sm, eq1], [gates])
        P.end()

    def phase_E1(self):
        P = self.P
        P.begin()
        wg = [P.sb([128, 8, 512], BF16, "wgE%d" % i) for i in range(2)]
        wu = [P.sb([128, 8, 512], BF16, "wuE%d" % i) for i in range(2)]
        sg = [P.sb([128, 512], F32, "sgE%d" % i) for i in range(2)]
        ast = [P.sb([128, 512], BF16, "astE%d" % i) for i in range(3)]
        bk = self.bank
        cnt = 0
        wc = 0
        for ex in range(NEXP):
            for fb in range(DFE // 512):
                g_, u_ = wg[wc % 2], wu[wc % 2]
                wc += 1
                P.dma(P.pool, g_, g_[:], self.moe_g, self.moe_g[ex][:, fb * 512:(fb + 1) * 512].rearrange("(kc k) j -> k kc j", k=128))
                P.dma(P.pool, u_, u_[:], self.moe_u, self.moe_u[ex][:, fb * 512:(fb + 1) * 512].rearrange("(kc k) j -> k kc j", k=128))
                for fcl in range(4):
                    fc = fb * 4 + fcl
                    fs = slice(fcl * 128, (fcl + 1) * 128)
                    for tg in range(4):
                        col0 = tg * 512
                        pg = bk[cnt % 2]
                        pu = bk[2 + cnt % 2]
                        s_ = sg[cnt % 2]
                        a_ = ast[cnt % 3]
                        cnt += 1
                        for kc in range(8):
                            P.op(P.pe, lambda e: e.matmul(pg[:], g_[:, kc, fs], self.hT[:, kc, col0:col0 + 512], start=(kc == 0), stop=(kc == 7)),
                                 [g_, self.hT], [pg], inc=(kc == 7))
                        for kc in range(8):
                            P.op(P.pe, lambda e: e.matmul(pu[:], u_[:, kc, fs], self.hT[:, kc, col0:col0 + 512], start=(kc == 0), stop=(kc == 7)),
                                 [u_, self.hT], [pu], inc=(kc == 7))
                        P.op(P.act, lambda e: e.activation(out=s_[:], in_=pg[:], func=AF.Silu), [pg], [s_])
                        P.op(P.dve, lambda e: e.tensor_tensor(out=a_[:], in0=s_[:], in1=pu[:], op=ALU.mult), [s_, pu], [a_])
                        P.dma(P.sp, self.ATE, self.ATE[ex, fc, :, col0:col0 + 512], a_, a_[:])
        P.end()

    def phase_E2(self, gates, xsrc_buf, xdst_fn):
        P = self.P
        P.begin()
        NFC = DFE // 128
        yacc = P.sb([128, 16, D], F32, "yacc")
        P.begin()
        HP = NFC // 2
        wd = [P.sb([128, HP, D], BF16, "wdE%d" % i) for i in range(3)]
        aT = [P.sb([128, HP, 128], BF16, "aTE%d" % i) for i in range(2)]
        bk = self.bank
        cnt = 0
        for ex in range(NEXP):
            for part in range(2):
                w = wd[(ex * 2 + part) % 3]
                for sp_ in range(2):
                    r0 = part * HP * 128 + sp_ * 7 * 128
                    P.dma(P.pool, w, w[:, sp_ * 7:(sp_ + 1) * 7, :], self.moe_d,
                          self.moe_d[ex][r0:r0 + 896, :].rearrange("(c f) j -> f c j", f=128))
                for n in range(16):
                    a_ = aT[cnt % 2]
                    zb = [bk[4 * (cnt % 2)], bk[4 * (cnt % 2) + 1]]
                    cnt += 1
                    P.dma(P.sp, a_, a_[:], self.ATE,
                          self.ATE[ex, part * HP:(part + 1) * HP, :, n * 128:(n + 1) * 128].rearrange("c f t -> f c t"))
                    for hf in range(2):
                        for fc in range(HP):
                            P.op(P.pe, lambda e: e.matmul(zb[hf][:], a_[:, fc, :], w[:, fc, hf * 512:(hf + 1) * 512], start=(fc == 0), stop=(fc == HP - 1)),
                                 [a_, w], [zb[hf]], inc=(fc == HP - 1))
                    for hf in range(2):
                        dst = yacc[:, n, hf * 512:(hf + 1) * 512]
                        if ex == 0 and part == 0:
                            P.op(P.dve, lambda e: e.tensor_scalar(out=dst, in0=zb[hf][:], scalar1=gates[:, n, ex:ex + 1], scalar2=None, op0=ALU.mult),
                                 [zb[hf], gates], [yacc])
                        else:
                            P.op(P.dve, lambda e: e.scalar_tensor_tensor(out=dst, in0=zb[hf][:], scalar=gates[:, n, ex:ex + 1], in1=dst,
                                                                         op0=ALU.mult, op1=ALU.add), [zb[hf], gates, yacc], [yacc])
        P.end()
        P.begin()
        xt = [P.sb([128, D], F32, "xE%d" % i) for i in range(2)]
        xo = [P.sb([128, D], F32, "xoE%d" % i) for i in range(2)]
        tmp = P.sb([128, D], F32, "tmpE")
        ss = P.sb([128, 4], F32, "ssE")
        for n in range(16):
            x_ = xt[n % 2]
            x_o = xo[n % 2]
            P.dma(P.sp, x_, x_[:], xsrc_buf, xsrc_buf[n * 128:(n + 1) * 128, :])
            self.resid([(yacc, yacc[:, n, 0:512]), (yacc, yacc[:, n, 512:1024])], x_, 0, 1, x_o, ss, tmp)
            db, dr = xdst_fn(n)
            P.dma(P.sp, db, db[dr:dr + 128, :], x_o, x_o[:])
        P.end()
        P.end()

    def layer(self, layer, last, xsrc, xdst):
        P = self.P
        full = not last
        PAR_BLK = {2, 7, 8, 9, 10}
        htiles = [(xsrc(n)[0], xsrc(n)[1], 1 if n in (16, 17) else 0, n * 128) for n in range(34)]
        ptiles = []
        for n in range(34):
            if n < 16:
                ptiles.append((n * 128, self.cs_own, n * 128, None))
            elif n < 18:
                ptiles.append((n * 128, None, 0, None))
            else:
                ptiles.append((n * 128, self.cs_par, (n - 18) * 128, None if full else PAR_BLK))
        self.phase_M(layer)
        P.begin()
        self.hT = P.sb([128, 8, NKEY], BF16, "hT")
        self.phase_H(htiles, 0)
        self.phase_P(layer, ptiles)
        P.end()
        groups = [(gi * 512, 512, list(range(34))) for gi in range(4)]
        if not last:
            groups.append((NLAT, 256, [16, 17]))
        if full:
            groups += [(NTOK + gi * 512, 512, list(range(34))) for gi in range(4)]
        self.phase_A(layer, groups)
        self.phase_G(layer, last, full)
        act = list(range(16)) + ([16, 17] if not last else []) + (list(range(18, 34)) if full else [])
        self.phase_X(layer, act, xsrc, self.X1)
        if layer % 2 == 0:
            P.begin()
            self.hT = P.sb([128, 8, NKEY], BF16, "hT")
            self.phase_H([(self.X1, n * 128, 1 if n in (16, 17) else 0, n * 128) for n in act], 1)
            fgroups = [(g * 512, 512) for g in range(4)]
            if not last:
                fgroups.append((NLAT, 256))
            if full:
                fgroups += [(NTOK + g * 512, 512) for g in range(4)]
            self.phase_F(self.X1, xdst, act, fgroups)
            P.end()
        else:
            gates = P.sb([128, 16, NEXP], F32, "gates")
            self.phase_R(self.X1, gates)
            P.begin()
            self.hT = P.sb([128, 8, NLAT], BF16, "hT")
            self.phase_H([(self.X1, i * 128, 0, i * 128) for i in range(16)], 1)
            self.phase_E1()
            P.end()
            self.phase_E2(gates, self.X1, xdst)

    def finish(self):
        P = self.P
        P.barrier()
        return self.nc


def own_tiles(xl, xc):
    t = [(xl, i * 128, 0, i * 128) for i in range(16)]
    t += [(xc, i * 128, 1, NLAT + i * 128) for i in range(2)]
    return t


def _rope_tables(pos):
    pos = np.asarray(pos)
    rows = (pos // 64).astype(np.float32)
    cols = (pos % 64).astype(np.float32)
    inv = (np.float32(10000.0) ** (-np.arange(32, dtype=np.float32) / np.float32(32))).astype(np.float32)
    ang = np.concatenate([rows[:, None] * inv, cols[:, None] * inv], axis=-1).astype(np.float32)
    ang = np.concatenate([ang, ang], axis=-1)
    cos = np.cos(ang).astype(np.float32)
    sin = np.sin(ang).astype(np.float32)
    sin[:, :64] *= -1.0
    return np.concatenate([cos, sin], axis=-1).astype(np.float32)


def _consts():
    c = np.zeros((128, 1024), np.float32)
    c[:, 0:128] = np.eye(128, dtype=np.float32)
    s = np.arange(128)[:, None]
    t = np.arange(128)[None, :]
    same = (s // 64) == (t // 64)
    ch0 = (t // 64) * 64
    for d, off in ((0, 128), (1, 512)):
        if d == 0:
            L = same & (s <= t)
            Lmid = same & (s <= ch0 + 31)
        else:
            L = same & (s >= t)
            Lmid = same & (s >= ch0 + 32)
        Ll = same
        c[:, off:off + 128] = L.astype(np.float32) - Lmid.astype(np.float32)
        c[:, off + 128:off + 256] = Ll.astype(np.float32) - L.astype(np.float32)
        c[:, off + 256:off + 384] = L.astype(np.float32)
    c[0:64, 896] = 1.0
    c[64:128, 897] = 1.0
    return c


def _rep(v):
    return np.broadcast_to(np.asarray(v, np.float32)[None, :], (128, v.shape[0]))


def _colchunks(v):
    return np.asarray(v, np.float32).reshape(8, 128).T


def prep_inputs(inp):
    x = inp["x"]; c = inp["c"]; ctx = inp["ctx"]; c_ctx = inp["c_ctx"]
    L = 2
    colv = np.zeros((L, 128, 48), np.float32)
    rowv = np.zeros((L, 128, 6528), np.float32)
    for l in range(L):
        bm = inp["b_mod"][l]
        colv[l, :, 0:8] = _colchunks(inp["pre_mix_norm"][l])
        colv[l, :, 8:16] = _colchunks(inp["pre_ffn_norm"][l])
        colv[l, :, 16:24] = _colchunks(bm[1024:2048])
        colv[l, :, 24:32] = _colchunks(bm[0:1024])
        colv[l, :, 32:40] = _colchunks(bm[4096:5120])
        colv[l, :, 40:48] = _colchunks(bm[3072:4096])
        rowv[l, :, 0:1024] = _rep(bm[2048:3072])
        rowv[l, :, 1024:2048] = _rep(bm[5120:6144])
        rowv[l, :, 2048:3072] = _rep(inp["post_mix_norm"][l])
        rowv[l, :, 3072:4096] = _rep(inp["post_ffn_norm"][l])
        rowv[l, :, 4096:4224] = _rep(inp["q_norm"][l])
        rowv[l, :, 4224:4352] = _rep(inp["k_norm"][l])
        rowv[l, :, 4352:4480] = _rep(inp["hg_norm"][l])
        rowv[l, :, 4480:5504] = _rep(inp["hg_lb_logits"][0])
        rowv[l, :, 5504:6528] = _rep(inp["hg_lb_logits"][1])
    consts = _consts()
    w_in0 = np.ascontiguousarray(inp["w_in"], np.float32)
    w_in1 = w_in0.copy()
    w_in1[:, :, 2560:3584] = w_in0[:, :, 3584:4608]
    w_in1[:, :, 3584:4608] = w_in0[:, :, 2560:3584]
    pos0 = np.arange(0, 2048)
    pos1 = np.arange(4095, 2047, -1)
    cs = [_rope_tables(pos0), _rope_tables(pos1)]
    shared = dict(
        consts=consts, w_mod=np.ascontiguousarray(inp["w_mod"], np.float32), colv=colv, rowv=rowv,
        w_att=np.ascontiguousarray(inp["w_att_branch"], np.float32),
        w_hg=np.ascontiguousarray(inp["w_hg_branch"], np.float32),
        w_out=np.ascontiguousarray(inp["w_out"], np.float32),
        ffn_g=np.ascontiguousarray(inp["ffn_w_gate"][0], np.float32),
        ffn_u=np.ascontiguousarray(inp["ffn_w_up"][0], np.float32),
        ffn_d=np.ascontiguousarray(inp["ffn_w_down"][0], np.float32),
        moe_r=np.ascontiguousarray(inp["moe_router"][0], np.float32),
        moe_g=np.ascontiguousarray(inp["moe_w_gate"][0], np.float32),
        moe_u=np.ascontiguousarray(inp["moe_w_up"][0], np.float32),
        moe_d=np.ascontiguousarray(inp["moe_w_down"][0], np.float32),
    )
    maps = []
    for core in range(8):
        b, half = core // 2, core % 2
        halves = [np.ascontiguousarray(x[b, :2048]), np.ascontiguousarray(x[b, 2048:][::-1])]
        cT = np.zeros((128, 16), np.float32)
        cT[:, 0::2] = _colchunks(c[b])
        cT[:, 1::2] = _colchunks(c_ctx)
        m = dict(shared)
        m.update(
            xl=halves[half], xp=halves[1 - half],
            xc=np.ascontiguousarray(ctx[b] if half == 0 else ctx[b][::-1]),
            cT=cT, cs_own=cs[half], cs_par=cs[1 - half],
            w_in=w_in0 if half == 0 else w_in1,
        )
        maps.append(m)
    return maps


def _run(nc, names, maps):
    ins = [{k: m[k] for k in names} for m in maps]
    res = run_bass_kernel_spmd(nc, ins, core_ids=list(range(8)))
    return res.results


def build_fused():
    b = Builder(moe=True, ctx_out=False)

    def src0(n):
        if n < 16:
            return (b.xl, n * 128)
        if n < 18:
            return (b.xc, (n - 16) * 128)
        return (b.xp, (n - 18) * 128)

    b.layer(0, False, src0, lambda n: (b.X2, n * 128))
    b.layer(1, True, lambda n: (b.X2, n * 128), lambda n: (b.yout, n * 128))
    return b


def kernel(**inputs):
    inputs = {k: np.asarray(v) for k, v in inputs.items()}
    maps = prep_inputs(inputs)
    b = build_fused()
    r = _run(b.finish(), b.in_names, maps)
    out = np.empty((4, 4096, D), np.float32)
    for bb in range(4):
        out[bb, :2048] = r[2 * bb]["yout"]
        out[bb, 2048:] = r[2 * bb + 1]["yout"][::-1]
    return out
```

```python
from contextlib import ExitStack

import numpy as np
import concourse.bass as bass
import concourse.mybir as mybir
from concourse.bass_utils import run_bass_kernel_spmd

F32 = mybir.dt.float32
BF16 = mybir.dt.bfloat16
U32 = mybir.dt.uint32
AF = mybir.ActivationFunctionType
ALU = mybir.AluOpType
AX = mybir.AxisListType

D = 1024
NLAT = 2048
NCTX = 256
NTOK = NLAT + NCTX
NKEY = NTOK + NLAT
PW = 8704
EPS = 1e-6
DFF = 2816
DFE = 3584
NEXP = 8
BLK_TYPES = ["q", "q", "kv", "hq", "hq", "fA", "fA", "fB", "fB", "hi", "hi", "hg", "hg",
             "ga", "ga", "gh", "gh"]


class Buf:
    def __init__(self, prog, ap, name, dram=False):
        self.p = prog
        self.ap = ap
        self.name = name
        self.dram = dram
        self.w = {}
        self.r = {}
        self.dsem = None

    def __getitem__(self, k):
        return self.ap[k]


class _View:
    def __init__(self, buf, ap):
        self.buf = buf
        self.ap = ap

    def __getitem__(self, k):
        return self.ap[k]


class Eng:
    def __init__(self, prog, e, name, fifo=False):
        self.p = prog
        self.e = e
        self.name = name
        self.sem = prog.nc.alloc_semaphore("tl_" + name)
        self.n = 0
        self.seen = {}
        self.fifo = fifo

    def wait(self, sem, val):
        if sem is self.sem and self.fifo:
            return
        k = id(sem)
        if self.seen.get(k, 0) >= val:
            return
        self.e.wait_ge(sem, val)
        self.seen[k] = val


class Prog:
    def __init__(self):
        nc = bass.Bass("TRN2", target_bir_lowering=False)
        self.nc = nc
        self.pe = Eng(self, nc.tensor, "pe", fifo=True)
        self.act = Eng(self, nc.scalar, "act")
        self.dve = Eng(self, nc.vector, "dve")
        self.pool = Eng(self, nc.gpsimd, "pool")
        self.sp = Eng(self, nc.sync, "sp")
        self.engs = [self.pe, self.act, self.dve, self.pool, self.sp]
        self.dma_pool = []
        self.dma_pools = {}
        self.dma_all = []
        self.nbuf = 0
        self.stack = None
        self.local = []
        self.scopes = []

    def sb(self, shape, dt, name=None):
        self.nbuf += 1
        name = (name or "sb") + "_%d" % self.nbuf
        if self.stack is not None:
            t = self.stack.enter_context(self.nc.sbuf_tensor(name, list(shape), dt))
            b = Buf(self, t[:], name)
            self.local.append(b)
            return b
        t = self.nc.alloc_sbuf_tensor(name, list(shape), dt)
        return Buf(self, t[:], name)

    def begin(self):
        self.scopes.append((self.stack, self.local))
        self.stack = ExitStack()
        self.local = []

    def end(self):
        self.barrier()
        self.release(self.local)
        self.stack.close()
        self.stack, self.local = self.scopes.pop()

    def dram(self, name, shape, dt, kind="Internal"):
        t = self.nc.dram_tensor(name, list(shape), dt, kind=kind)
        return Buf(self, t.ap(), name, dram=True)

    def sub(self, ap, name="sub"):
        return Buf(self, ap, name)

    def _dsem(self, buf, kind):
        if buf.dsem is None:
            buf.dsem = {}
        if kind not in buf.dsem:
            pool = self.dma_pools.setdefault(kind, [])
            if pool:
                buf.dsem[kind] = pool.pop()
            else:
                ent = [self.nc.alloc_semaphore("dm%s%d" % (kind, len(self.dma_all))), 0]
                self.dma_all.append(ent)
                buf.dsem[kind] = ent
        return buf.dsem[kind]

    def release(self, bufs):
        for b in bufs:
            if b.dsem:
                for kind, ent in b.dsem.items():
                    self.dma_pools.setdefault(kind, []).append(ent)
                b.dsem = None

    def _deps(self, eng, reads, writes):
        for b in reads:
            if b.dram:
                continue
            for (s, v) in b.w.values():
                eng.wait(s, v)
        for b in writes:
            if b.dram:
                continue
            for (s, v) in b.w.values():
                eng.wait(s, v)
            for (s, v) in b.r.values():
                eng.wait(s, v)

    def _record(self, tok, reads, writes):
        s, v = tok
        k = id(s)
        for b in reads:
            o = b.r.get(k)
            if o is None or o[1] < v:
                b.r[k] = (s, v)
        for b in writes:
            if b.dram:
                o = b.w.get(k)
                if o is None or o[1] < v:
                    b.w[k] = (s, v)
            else:
                b.w = {k: (s, v)}
                b.r = {}

    def op(self, eng, fn, reads=(), writes=(), inc=True):
        self._deps(eng, reads, writes)
        ins = fn(eng.e)
        if inc:
            eng.n += 1
            ins.then_inc(eng.sem, 1)
            tok = (eng.sem, eng.n)
        else:
            tok = (eng.sem, eng.n + 1)
        self._record(tok, reads, writes)
        return tok

    def dma(self, q, out_buf, out_ap, in_buf, in_ap):
        self._deps(q, [in_buf], [out_buf])
        sbside = in_buf if out_buf.dram else out_buf
        ent = self._dsem(sbside, "sw" if q is self.pool else "hw")
        ins = q.e.dma_start(out=out_ap, in_=in_ap)
        ent[1] += 16
        ins.then_inc(ent[0], 16)
        tok = (ent[0], ent[1])
        self._record(tok, [in_buf], [out_buf])
        return tok

    def barrier(self, bufs=()):
        toks = [(e.sem, e.n) for e in self.engs if e.n > 0]
        for ent in self.dma_all:
            if ent[1] > 0:
                toks.append((ent[0], ent[1]))
        for e in self.engs:
            for (s, v) in toks:
                if s is not e.sem:
                    e.wait(s, v)
        for b in bufs:
            b.w = {}
            b.r = {}


class Builder:
    def __init__(self, layers=(0, 1), stop_after=None, dump=(), moe=True, ctx_out=True):
        self.P = Prog()
        self.nc = self.P.nc
        self.layers = layers
        self.stop_after = stop_after
        self.dump = set(dump)
        P = self.P
        nc = self.nc
        self.in_names = []

        def ein(n, s, dt=F32):
            self.in_names.append(n)
            return P.dram(n, s, dt, kind="ExternalInput")
        self.xl = ein("xl", [NLAT, D])
        self.xp = ein("xp", [NLAT, D])
        self.xc = ein("xc", [NCTX, D])
        self.cT = ein("cT", [128, 16])
        self.cs_own = ein("cs_own", [NLAT, 256])
        self.cs_par = ein("cs_par", [NLAT, 256])
        self.consts = ein("consts", [128, 1024])
        self.w_mod = ein("w_mod", [2, D, 6 * D])
        self.colv = ein("colv", [2, 128, 48])
        self.rowv = ein("rowv", [2, 128, 4096 + 384 + 2048])
        self.w_in = ein("w_in", [2, D, PW])
        self.w_att = ein("w_att", [2, D, D])
        self.w_hg = ein("w_hg", [2, D, D])
        self.w_out = ein("w_out", [2, D, D])
        self.ffn_g = ein("ffn_g", [D, DFF])
        self.ffn_u = ein("ffn_u", [D, DFF])
        self.ffn_d = ein("ffn_d", [DFF, D])
        if moe:
            self.moe_r = ein("moe_r", [D, NEXP])
            self.moe_g = ein("moe_g", [NEXP, D, DFE])
            self.moe_u = ein("moe_u", [NEXP, D, DFE])
            self.moe_d = ein("moe_d", [NEXP, DFE, D])
        self.yout = P.dram("yout", [NLAT, D], F32, kind="ExternalOutput")
        self.xcout = P.dram("xcout", [NCTX, D], F32, kind="ExternalOutput") if ctx_out else None

        def scr(n, s, dt):
            return P.dram(n, s, dt, kind="ExternalOutput" if n in self.dump else "Internal")
        self.QT = scr("QT", [8, 128, NKEY], BF16)
        self.KT = scr("KT", [2, 128, NKEY], BF16)
        self.V = scr("V", [NKEY, 256], BF16)
        self.HQ = scr("HQ", [NKEY, D], BF16)
        self.KA = scr("KA", [NKEY, D], BF16)
        self.KB = scr("KB", [NKEY, D], BF16)
        self.LFA = scr("LFA", [NKEY, D], F32)
        self.LFB = scr("LFB", [NKEY, D], F32)
        self.HI = scr("HI", [NKEY, D], BF16)
        self.HG = scr("HG", [NKEY, D], BF16)
        self.GA = scr("GA", [NKEY, D], BF16)
        self.GH = scr("GH", [NKEY, D], BF16)
        self.OA = scr("OA", [NKEY, D], F32)
        self.OB = scr("OB", [NKEY, D], F32)
        self.ATT = scr("ATT", [8, 128, NKEY], BF16)
        self.AT = scr("AT", [22, 128, NKEY], BF16)
        if moe:
            self.ATE = scr("ATE", [NEXP, 28, 128, NLAT], BF16)
        self.X1 = scr("X1", [NKEY, D], F32)
        self.X2 = scr("X2", [NKEY, D], F32)
        self.HTD = scr("HTD", [128, 8, NKEY], BF16)

        self.bank = [Buf(P, nc.alloc_psum_tensor("bank%d" % i, [128, 512], F32)[:], "bank%d" % i)
                     for i in range(8)]

        self.c_f32 = P.sb([128, 1024], F32, "consts")
        self.ident_bf = P.sb([128, 128], BF16, "identbf")
        self.ones_bf = P.sb([128, 128], BF16, "onesbf")
        P.dma(P.sp, self.c_f32, self.c_f32[:], self.consts, self.consts[:])
        P.op(P.dve, lambda e: e.tensor_copy(out=self.ident_bf[:], in_=self.c_f32[:, 0:128]),
             [self.c_f32], [self.ident_bf])
        P.op(P.dve, lambda e: e.memset(self.ones_bf[:], 1.0), [], [self.ones_bf])
        self.ident_f = self.c_f32[:, 0:128]
        self.AB = P.sb([128, 4, 8, 2], F32, "AB")
        self.grow = P.sb([128, 2, 2, 1024], F32, "grow")
        self.hT = None

    def wblock_load(self, wbuf, src_ap):
        P = self.P
        return src_ap.rearrange("(kc k) j -> k kc j", k=128)

    def phase_M(self, layer):
        P = self.P
        P.begin()
        cT = P.sb([128, 8, 2], F32, "cTf")
        cs = P.sb([128, 8, 2], BF16, "csbf")
        crep = P.sb([128, 2, 8, 128], BF16, "crep")
        colv = P.sb([128, 48], F32, "colv")
        rows = P.sb([128, 4096], F32, "rows")
        wb = [P.sb([128, 8, 512], BF16, "wmod%d" % i) for i in range(3)]
        modc = P.sb([128, 4, 8, 2], F32, "modc")
        tmp = P.sb([128, 8, 2], F32, "tmpM")
        P.dma(P.sp, cT, cT[:], self.cT, self.cT[:].rearrange("p (k x) -> p k x", x=2))
        P.dma(P.sp, colv, colv[:], self.colv, self.colv[layer])
        P.dma(P.sp, rows, rows[:], self.rowv, self.rowv[layer][:, 0:4096])
        P.op(P.act, lambda e: e.activation(out=cs[:], in_=cT[:], func=AF.Silu), [cT], [cs])
        for x in range(2):
            P.op(P.dve, lambda e: e.tensor_copy(out=crep[:, x], in_=cs[:, :, x:x + 1].to_broadcast([128, 8, 128])),
                 [cs], [crep])
        psM = self.bank[0]
        psG = [self.bank[1], self.bank[2]]
        for cb in range(12):
            w = wb[cb % 3]
            src = self.w_mod[layer][:, cb * 512:(cb + 1) * 512].rearrange("(kc k) j -> k kc j", k=128)
            P.dma(P.pool, w, w[:], self.w_mod, src)
            slot, half = cb // 2, cb % 2
            if slot in (0, 1, 3, 4):
                sidx = {0: 1, 1: 0, 3: 3, 4: 2}[slot]
                for j in range(4):
                    col = (sidx * 8 + half * 4 + j) * 2
                    for kc in range(8):
                        P.op(P.pe, lambda e: e.matmul(psM[:, col:col + 2], w[:, kc, j * 128:(j + 1) * 128],
                                                      cs[:, kc, :], start=(kc == 0), stop=(kc == 7)),
                             [w, cs], [psM], inc=(kc == 7))
            else:
                gi = 0 if slot == 2 else 1
                for x in range(2):
                    for kc in range(8):
                        P.op(P.pe, lambda e: e.matmul(psG[x][:], crep[:, x, kc, :], w[:, kc, :],
                                                      start=(kc == 0), stop=(kc == 7)),
                             [w, crep], [psG[x]], inc=(kc == 7))
                    dst = self.grow[:, x, gi, half * 512:(half + 1) * 512]
                    P.op(P.dve, lambda e: e.tensor_tensor(out=dst, in0=psG[x][:],
                                                          in1=rows[:, gi * 1024 + half * 512: gi * 1024 + (half + 1) * 512],
                                                          op=ALU.add), [psG[x], rows], [self.grow])
                    P.op(P.dve, lambda e: e.tensor_tensor(out=dst, in0=dst,
                                                          in1=rows[:, (2 + gi) * 1024 + half * 512: (2 + gi) * 1024 + (half + 1) * 512],
                                                          op=ALU.mult), [rows, self.grow], [self.grow])
        P.op(P.dve, lambda e: e.tensor_tensor(
            out=modc[:], in0=psM[:, 0:64].rearrange("p (s k x) -> p s k x", s=4, k=8),
            in1=colv[:, 16:48].rearrange("p (s k) -> p s k", s=4).unsqueeze(3).to_broadcast([128, 4, 8, 2]),
            op=ALU.add), [psM, colv], [modc])
        for i in range(2):
            P.op(P.dve, lambda e: e.tensor_scalar(out=tmp[:], in0=modc[:, 2 * i], scalar1=1.0, scalar2=None,
                                                  op0=ALU.add), [modc], [tmp])
            P.op(P.dve, lambda e: e.tensor_tensor(out=self.AB[:, 2 * i], in0=tmp[:],
                                                  in1=colv[:, 8 * i:8 * i + 8].unsqueeze(2).to_broadcast([128, 8, 2]),
                                                  op=ALU.mult), [tmp, colv], [self.AB])
            P.op(P.dve, lambda e: e.tensor_copy(out=self.AB[:, 2 * i + 1], in_=modc[:, 2 * i + 1]),
                 [modc], [self.AB])
        P.end()

    def phase_H(self, tiles, which):
        P = self.P
        P.begin()
        xt = [P.sb([128, D], F32, "xt%d" % i) for i in range(3)]
        xs = [P.sb([128, D], BF16, "xs%d" % i) for i in range(2)]
        junk = P.sb([128, D], BF16, "junk")
        ss = [P.sb([128, 2], F32, "ss%d" % i) for i in range(2)]
        psT = [self.bank[0], self.bank[1]]

        def stage_a(n):
            src, row0, xidx, col = tiles[n]
            a = xt[n % 3]
            b = xs[n % 2]
            s_ = ss[n % 2]
            P.op(P.act, lambda e: e.activation(out=junk[:], in_=a[:], func=AF.Square, accum_out=s_[:, 0:1]),
                 [a], [junk, s_])
            P.op(P.act, lambda e: e.activation(out=s_[:, 1:2], in_=s_[:, 0:1], func=AF.Sqrt, scale=1.0 / D, bias=EPS),
                 [s_], [s_])
            P.op(P.dve, lambda e: e.reciprocal(out=s_[:, 0:1], in_=s_[:, 1:2]), [s_], [s_])
            P.op(P.dve, lambda e: e.tensor_scalar(out=b[:], in0=a[:], scalar1=s_[:, 0:1], scalar2=None, op0=ALU.mult),
                 [a, s_], [b])

        def stage_b(n):
            src, row0, xidx, col = tiles[n]
            b = xs[n % 2]
            pt = psT[n % 2]
            ptb = pt[:].bitcast(BF16)
            for kc in range(8):
                P.op(P.pe, lambda e: e.transpose(out=ptb[:, kc * 128:(kc + 1) * 128], in_=b[:, kc * 128:(kc + 1) * 128],
                                                 identity=self.ident_bf[:]), [b, self.ident_bf], [pt], inc=(kc == 7))
            for kc in range(8):
                eng = P.dve
                if eng is P.act:
                    P.op(eng, lambda e: e.activation(out=self.hT[:, kc, col:col + 128], in_=ptb[:, kc * 128:(kc + 1) * 128],
                                                     func=AF.Identity, scale=self.AB[:, 2 * which, kc, xidx:xidx + 1],
                                                     bias=self.AB[:, 2 * which + 1, kc, xidx:xidx + 1]),
                         [pt, self.AB], [])
                else:
                    P.op(eng, lambda e: e.tensor_scalar(out=self.hT[:, kc, col:col + 128], in0=ptb[:, kc * 128:(kc + 1) * 128],
                                                        scalar1=self.AB[:, 2 * which, kc, xidx:xidx + 1],
                                                        scalar2=self.AB[:, 2 * which + 1, kc, xidx:xidx + 1],
                                                        op0=ALU.mult, op1=ALU.add),
                         [pt, self.AB], [])

        def load(n):
            src, row0, xidx, col = tiles[n]
            P.dma(P.sp, xt[n % 3], xt[n % 3][:], src, src[row0:row0 + 128, :])

        NT = len(tiles)
        load(0)
        if NT > 1:
            load(1)
        stage_a(0)
        for n in range(NT):
            if n + 2 < NT:
                load(n + 2)
            if n + 1 < NT:
                stage_a(n + 1)
            stage_b(n)
        P.end()

    def phase_P(self, layer, tiles):
        P = self.P
        P.begin()
        wb = [P.sb([128, 8, 512], BF16, "win%d" % i) for i in range(3)]
        rows = P.sb([128, 2432], F32, "rowsP")
        oml = P.sb([128, 1024], F32, "oml")
        cs_all = {}
        for nm, src in (("own", self.cs_own), ("par", self.cs_par)):
            cs_all[nm] = P.sb([128, 16, 256], F32, "cs_" + nm)
            P.dma(P.sp, cs_all[nm], cs_all[nm][:], src, src[:].rearrange("(n p) c -> p n c", p=128))
        sq = P.sb([128, 512], F32, "sq")
        ss = P.sb([128, 8], F32, "ssP")
        qn = P.sb([128, 512], F32, "qn")
        t1 = P.sb([128, 512], F32, "t1")
        t2 = P.sb([128, 512], F32, "t2")
        qr = [P.sb([128, 512], BF16, "qr%d" % i) for i in range(2)]
        stT = [P.sb([128, 512], BF16, "stT%d" % i) for i in range(2)]
        stb = [P.sb([128, 512], BF16, "stb%d" % i) for i in range(3)]
        stf = [P.sb([128, 512], F32, "stf%d" % i) for i in range(4)]
        kkfs = [P.sb([128, 512], F32, "kkf%d" % i) for i in range(8)]
        P.dma(P.sp, rows, rows[:], self.rowv, self.rowv[layer][:, 4096:6528])
        if layer == 1:
            P.op(P.dve, lambda e: e.tensor_tensor(out=oml[:], in0=rows[:, 384:1408], in1=rows[:, 1408:2432],
                                                  op=ALU.subtract), [rows], [oml])
            P.op(P.act, lambda e: e.activation(out=oml[:], in_=oml[:], func=AF.Sigmoid), [oml], [oml])
        blocks = list(range(17))
        cnt = {"mm": 0, "tr": 0, "b": 0, "f": 0, "T": 0, "q": 0, "cs": 0, "kk": 0}

        def nxt(k, lst):
            v = lst[cnt[k] % len(lst)]
            cnt[k] += 1
            return v

        sq2 = [sq, P.sb([128, 512], F32, "sqb")]
        ss2 = [ss, P.sb([128, 8], F32, "ssPb")]
        qn2 = [qn, P.sb([128, 512], F32, "qnb")]

        def qk_block(typ, sub, w, items):
            nh = 4 if typ == "q" else 2
            wd_ = nh * 128
            gain = rows[:, 0:128] if typ == "q" else rows[:, 128:256]
            v3 = lambda ap: ap[:, :wd_].rearrange("p (h d) -> p h d", h=nh)
            gb = gain.unsqueeze(1).to_broadcast([128, nh, 128])
            ctx = {}

            def mm(i):
                srow, cs_src, cs_row = items[i]
                pb = nxt("mm", self.bank[0:4])
                for kc in range(8):
                    P.op(P.pe, lambda e: e.matmul(pb[:], self.hT[:, kc, srow:srow + 128], w[:, kc, :],
                                                  start=(kc == 0), stop=(kc == 7)), [self.hT, w], [pb], inc=(kc == 7))
                cst = None
                if cs_src is not None:
                    cbuf = cs_all["own" if cs_src is self.cs_own else "par"]
                    cst = _View(cbuf, cbuf[:, cs_row // 128, :])
                ctx[i] = dict(pb=pb, cst=cst, q_out=qr[i % 2])

            def s1a(i):
                pb = ctx[i]["pb"]
                sq_, ss_ = sq2[i % 2], ss2[i % 2]
                P.op(P.act, lambda e: e.activation(out=sq_[:, :wd_], in_=pb[:, :wd_], func=AF.Square), [pb], [sq_])
                P.op(P.dve, lambda e: e.tensor_reduce(out=ss_[:, 0:nh], in_=sq_[:, :wd_].rearrange("p (h d) -> p h d", h=nh),
                                                      axis=AX.X, op=ALU.add), [sq_], [ss_])
                P.op(P.act, lambda e: e.activation(out=ss_[:, 4:4 + nh], in_=ss_[:, 0:nh], func=AF.Sqrt, scale=1.0 / 128, bias=EPS),
                     [ss_], [ss_])

            def s1b(i):
                pb, cst, q_out = ctx[i]["pb"], ctx[i]["cst"], ctx[i]["q_out"]
                ss_, qn_ = ss2[i % 2], qn2[i % 2]
                P.op(P.dve, lambda e: e.reciprocal(out=ss_[:, 0:nh], in_=ss_[:, 4:4 + nh]), [ss_], [ss_])
                P.op(P.dve, lambda e: e.tensor_tensor(out=v3(qn_), in0=pb[:, :wd_].rearrange("p (h d) -> p h d", h=nh),
                                                      in1=ss_[:, 0:nh].unsqueeze(2).to_broadcast([128, nh, 128]), op=ALU.mult),
                     [pb, ss_], [qn_])
                if cst is not None:
                    P.op(P.dve, lambda e: e.tensor_tensor(out=v3(qn_), in0=v3(qn_), in1=gb, op=ALU.mult), [qn_, rows], [qn_])
                else:
                    P.op(P.dve, lambda e: e.tensor_tensor(out=v3(q_out), in0=v3(qn_), in1=gb, op=ALU.mult), [qn_, rows], [q_out])

            def s1c(i):
                cst, q_out = ctx[i]["cst"], ctx[i]["q_out"]
                qn_ = qn2[i % 2]
                if cst is None:
                    return
                P.op(P.dve, lambda e: e.tensor_tensor(out=v3(t2)[:, :, 0:64], in0=v3(qn_)[:, :, 64:128],
                                                       in1=cst[:, 128:192].unsqueeze(1).to_broadcast([128, nh, 64]), op=ALU.mult),
                     [qn_, cst.buf], [t2])
                P.op(P.dve, lambda e: e.tensor_tensor(out=v3(t2)[:, :, 64:128], in0=v3(qn_)[:, :, 0:64],
                                                       in1=cst[:, 192:256].unsqueeze(1).to_broadcast([128, nh, 64]), op=ALU.mult),
                     [qn_, cst.buf, t2], [t2])
                P.op(P.dve, lambda e: e.tensor_tensor(out=v3(t1), in0=v3(qn_),
                                                      in1=cst[:, 0:128].unsqueeze(1).to_broadcast([128, nh, 128]), op=ALU.mult),
                     [qn_, cst.buf], [t1])
                P.op(P.dve, lambda e: e.tensor_tensor(out=q_out[:, :wd_], in0=t1[:, :wd_], in1=t2[:, :wd_], op=ALU.add),
                     [t1, t2], [q_out])

            def s2(i):
                srow = items[i][0]
                pb, q_out = ctx[i]["pb"], ctx[i]["q_out"]
                pt = nxt("tr", [self.bank[4], self.bank[5]])
                ptb = pt[:].bitcast(BF16)
                for h in range(nh):
                    P.op(P.pe, lambda e: e.transpose(out=ptb[:, h * 128:(h + 1) * 128], in_=q_out[:, h * 128:(h + 1) * 128],
                                                     identity=self.ident_bf[:]), [q_out, self.ident_bf], [pt], inc=(h == nh - 1))
                st = nxt("T", stT)
                P.op(P.act, lambda e: e.copy(out=st[:, :wd_], in_=ptb[:, :wd_]), [pt], [st])
                if typ == "q":
                    dbuf, dap = self.QT, self.QT[sub * 4:sub * 4 + 4, :, srow:srow + 128].rearrange("h d t -> d h t")
                else:
                    dbuf, dap = self.KT, self.KT[:, :, srow:srow + 128].rearrange("h d t -> d h t")
                P.dma(P.sp, dbuf, dap, st, st[:, :wd_].rearrange("p (h t) -> p h t", h=nh))
                if typ == "kv":
                    sv = nxt("b", stb)
                    P.op(P.act, lambda e: e.copy(out=sv[:, :256], in_=pb[:, 256:512]), [pb], [sv])
                    P.dma(P.sp, self.V, self.V[srow:srow + 128, :], sv, sv[:, :256])
                del ctx[i]

            N = len(items)
            mm(0)
            s1a(0)
            s1b(0)
            for i in range(N):
                if i + 1 < N:
                    mm(i + 1)
                    s1a(i + 1)
                s1c(i)
                s2(i)
                if i + 1 < N:
                    s1b(i + 1)

        def simple(psv, func, scale, dst_buf, dst_ap):
            st = nxt("b", stb)
            if func is None:
                P.op(P.act, lambda e: e.copy(out=st[:, :psv.shape[1]], in_=psv), [psb], [st])
            else:
                P.op(P.act, lambda e: e.activation(out=st[:, :psv.shape[1]], in_=psv, func=func, scale=scale), [psb], [st])
            P.dma(P.sp, dst_buf, dst_ap, st, st[:, :psv.shape[1]])

        pending = []

        def flush_ln():
            for (kkf_, ld_, rs_, c0_) in pending:
                sf = nxt("f", stf)
                P.op(P.act, lambda e: e.activation(out=sf[:], in_=kkf_[:], func=AF.Ln, scale=-1.0, bias=1.0), [kkf_], [sf])
                P.dma(P.sp, ld_, ld_[rs_, c0_:c0_ + 512], sf, sf[:])
            del pending[:]

        for blk in blocks:
            typ = BLK_TYPES[blk]
            w = nxt("b", [wb[0]]) if False else wb[blocks.index(blk) % 3]
            src = self.w_in[layer][:, blk * 512:(blk + 1) * 512].rearrange("(kc k) j -> k kc j", k=128)
            P.dma(P.pool, w, w[:], self.w_in, src)
            sub = blk % 2 if blk != 2 else 0
            if blk <= 1:
                sub = blk
            elif blk >= 3:
                sub = (blk - 3) % 2
            if typ in ("q", "kv"):
                qk_block(typ, sub, w, [(t_[0], t_[1], t_[2]) for t_ in tiles if t_[3] is None or blk in t_[3]])
                continue
            for (srow, cs_src, cs_row, tblocks) in tiles:
                if tblocks is not None and blk not in tblocks:
                    continue
                hcol = srow
                is_lat = cs_src is not None
                psb = nxt("mm", self.bank[0:4])
                for kc in range(8):
                    P.op(P.pe, lambda e: e.matmul(psb[:], self.hT[:, kc, hcol:hcol + 128], w[:, kc, :],
                                                  start=(kc == 0), stop=(kc == 7)), [self.hT, w], [psb], inc=(kc == 7))
                c0 = sub * 512
                rs = slice(srow, srow + 128)
                if False:
                    pass
                elif typ == "hq":
                    simple(psb[:], AF.Silu, 1.0, self.HQ, self.HQ[rs, c0:c0 + 512])
                elif typ == "hg":
                    simple(psb[:], AF.Silu, 1.0, self.HG, self.HG[rs, c0:c0 + 512])
                elif typ == "ga":
                    simple(psb[:], AF.Sigmoid, 1.0, self.GA, self.GA[rs, c0:c0 + 512])
                elif typ == "gh":
                    simple(psb[:], AF.Sigmoid, 1.0, self.GH, self.GH[rs, c0:c0 + 512])
                elif typ == "hi":
                    simple(psb[:], None, None, self.HI, self.HI[rs, c0:c0 + 512])
                else:
                    kd, ld = (self.KA, self.LFA) if typ == "fA" else (self.KB, self.LFB)
                    kkf = nxt("kk", kkfs)
                    P.op(P.act, lambda e: e.activation(out=kkf[:], in_=psb[:], func=AF.Sigmoid, scale=-1.0), [psb], [kkf])
                    if layer == 1:
                        P.op(P.dve, lambda e: e.tensor_tensor(out=kkf[:], in0=kkf[:], in1=oml[:, c0:c0 + 512], op=ALU.mult),
                             [kkf, oml], [kkf])
                    st = nxt("b", stb)
                    P.op(P.pool, lambda e: e.tensor_copy(out=st[:], in_=kkf[:]), [kkf], [st])
                    P.dma(P.sp, kd, kd[rs, c0:c0 + 512], st, st[:])
                    pending.append((kkf, ld, rs, c0))
                    if len(pending) == 4:
                        flush_ln()
            flush_ln()
        P.end()

    def phase_A(self, layer, groups):
        P = self.P
        P.begin()
        KTall = P.sb([128, 2, NKEY], BF16, "KTall")
        Vall = P.sb([128, 34, 256], BF16, "Vall")
        qt = [P.sb([128, 8, 512], BF16, "qtA%d" % i) for i in range(2)]
        pT = [P.sb([128, 512], BF16, "pT%d" % i) for i in range(4)]
        rden = P.sb([128, 512], F32, "rden")
        ost = [P.sb([128, 512], BF16, "ost%d" % i) for i in range(2)]
        for g in range(2):
            P.dma(P.sp, KTall, KTall[:, g, :], self.KT, self.KT[g])
        P.dma(P.sp, Vall, Vall[:], self.V, self.V[:].rearrange("(kb p) c -> p kb c", p=128))
        scale = 1.0 / float(np.sqrt(128.0))
        cnt = 0
        for gidx, (col0, n, kbs) in enumerate(groups):
            q = qt[gidx % 2]
            P.dma(P.sp, q, q[:, :, :n], self.QT, self.QT[:, :, col0:col0 + n].rearrange("h d t -> d h t"))
            for h in range(8):
                g = h // 4
                pso = self.bank[2 + (cnt % 2)]
                psd = self.bank[4 + (cnt % 2)]
                o_st = ost[cnt % 2]
                cnt += 1

                sbanks = [self.bank[0], self.bank[1], self.bank[6]]

                def S(i):
                    kb = kbs[i]
                    ps = sbanks[i % 3]
                    P.op(P.pe, lambda e: e.matmul(ps[:, :n], KTall[:, g, kb * 128:(kb + 1) * 128], q[:, h, :n],
                                                  start=True, stop=True), [KTall, q], [ps])
                S(0)
                if len(kbs) > 1:
                    S(1)
                for i, kb in enumerate(kbs):
                    if i + 2 < len(kbs):
                        S(i + 2)
                    ps = sbanks[i % 3]
                    p = pT[i % 4]
                    P.op(P.act, lambda e: e.activation(out=p[:, :n], in_=ps[:, :n], func=AF.Exp, scale=scale), [ps], [p])
                    first, lst = (i == 0), (i == len(kbs) - 1)
                    P.op(P.pe, lambda e: e.matmul(pso[:, :n], Vall[:, kb, g * 128:(g + 1) * 128], p[:, :n],
                                                  start=first, stop=lst), [Vall, p], [pso], inc=False)
                    P.op(P.pe, lambda e: e.matmul(psd[:, :n], self.ones_bf[:], p[:, :n],
                                                  start=first, stop=lst), [self.ones_bf, p], [psd])
                P.op(P.dve, lambda e: e.reciprocal(out=rden[:, :n], in_=psd[:, :n]), [psd], [rden])
                P.op(P.dve, lambda e: e.tensor_tensor(out=o_st[:, :n], in0=pso[:, :n], in1=rden[:, :n], op=ALU.mult),
                     [pso, rden], [o_st])
                P.dma(P.sp, self.ATT, self.ATT[h, :, col0:col0 + n], o_st, o_st[:, :n])
        P.end()

    def phase_G(self, layer, last, full):
        P = self.P
        P.begin()
        c = self.c_f32
        M = {0: (c[:, 128:256], c[:, 256:384], c[:, 384:512]), 1: (c[:, 512:640], c[:, 640:768], c[:, 768:896])}
        ind = c[:, 896:898]
        mask = [P.sb([128, 128], U32, "mask%d" % d) for d in range(2)]
        for d in range(2):
            P.op(P.dve, lambda e: e.tensor_scalar(out=mask[d][:], in0=M[d][2], scalar1=0.5, scalar2=None, op0=ALU.is_gt),
                 [c], [mask[d]])
        S = [P.sb([128, 128], F32, "S%d" % h) for h in range(8)]
        Sb = [P.sb([128, 128], BF16, "Sb%d" % h) for h in range(8)]
        Am = [[P.sb([128, 4, 128], BF16, "Am%d%d" % (d, i)) for i in range(2)] for d in range(2)]
        qhf = [[P.sb([128, 4, 128], BF16, "qhf%d%d" % (d, i)) for i in range(2)] for d in range(2)]
        qhs = [[P.sb([128, 4, 128], BF16, "qhs%d%d" % (d, i)) for i in range(2)] for d in range(2)]
        for grp in (Am, qhf, qhs):
            for d in range(2):
                for b_ in grp[d]:
                    P.op(P.dve, lambda e: e.memset(b_[:], 0.0), [], [b_])
        ld = [dict(kk=P.sb([128, D], BF16, "kk%d" % i), lf=P.sb([128, D], F32, "lf%d" % i),
                   hi=P.sb([128, D], BF16, "hi%d" % i), hq=P.sb([128, D], BF16, "hq%d" % i)) for i in range(2)]
        e1 = P.sb([128, 512], F32, "e1"); e1n = P.sb([128, 512], F32, "e1n")
        e3 = P.sb([128, 512], F32, "e3"); e2 = P.sb([128, 512], F32, "e2")
        dec = [P.sb([128, 8], F32, "dec%d" % i) for i in range(2)]
        qt = P.sb([128, 512], BF16, "qt"); kt = P.sb([128, 512], BF16, "kt")
        qh = P.sb([128, 512], BF16, "qh")
        kh = [P.sb([128, 512], BF16, "kh%d" % i) for i in range(2)]
        qtT = P.sb([128, 512], BF16, "qtT"); ktT = P.sb([128, 512], BF16, "ktT")
        ostage = [P.sb([128, D], F32, "ostage%d" % i) for i in range(2)]
        bk = self.bank

        def reset_state():
            for h in range(8):
                P.op(P.dve, lambda e: e.memset(S[h][:], 0.0), [], [S[h]])
                P.op(P.dve, lambda e: e.memset(Sb[h][:], 0.0), [], [Sb[h]])

        plan = []

        def tile(d, row0, srcK, srcLF, full, outd):
            plan.append(("tile", d, row0, srcK, srcLF, full, outd))

        def tile_loads(idx, d, row0, srcK, srcLF, full, outd):
            L = ld[idx % 2]
            rs = slice(row0, row0 + 128)
            P.dma(P.sp, L["kk"], L["kk"][:], srcK, srcK[rs, :])
            P.dma(P.sp, L["lf"], L["lf"][:], srcLF, srcLF[rs, :])
            P.dma(P.sp, L["hi"], L["hi"][:], self.HI, self.HI[rs, :])
            if full:
                P.dma(P.sp, L["hq"], L["hq"][:], self.HQ, self.HQ[rs, :])

        def front(u, idx, half, d, row0, srcK, srcLF, full, outd):
            L = ld[idx % 2]
            kk, lf, hi, hq = L["kk"], L["lf"], L["hi"], L["hq"]
            fc, sc = (0, 1) if d == 0 else (1, 0)
            fr = slice(fc * 64, fc * 64 + 64)
            sr = slice(sc * 64, sc * 64 + 64)
            M1, M2, M3 = M[d]
            pu = u % 2
            c0 = half * 512
            cs_ = slice(c0, c0 + 512)
            P.op(P.pe, lambda e: e.matmul(bk[1][:], M2, lf[:, cs_], start=True, stop=True), [c, lf], [bk[1]])
            psD = bk[4][:, 256:264]
            for hh in range(4):
                P.op(P.pe, lambda e: e.matmul(psD[:, hh * 2:hh * 2 + 2], lf[:, c0 + hh * 128:c0 + (hh + 1) * 128], ind,
                                              start=True, stop=True), [c, lf], [bk[4]], inc=(hh == 3))
            if full:
                P.op(P.pe, lambda e: e.matmul(bk[0][:], M1, lf[:, cs_], start=True, stop=True), [c, lf], [bk[0]])
            P.op(P.act, lambda e: e.activation(out=e2[:], in_=bk[1][:], func=AF.Exp), [bk[1]], [e2])
            P.op(P.act, lambda e: e.activation(out=dec[pu][:], in_=psD, func=AF.Exp), [bk[4]], [dec[pu]])
            if full:
                P.op(P.pe, lambda e: e.matmul(bk[1][:], M3, lf[:, cs_], start=True, stop=True), [c, lf], [bk[1]])
            P.op(P.dve, lambda e: e.tensor_tensor(out=kh[pu][:], in0=kk[:, cs_], in1=e2[:], op=ALU.mult), [kk, e2], [kh[pu]])
            if not full:
                return
            P.op(P.act, lambda e: e.activation(out=e1[:], in_=bk[0][:], func=AF.Exp), [bk[0]], [e1])
            P.op(P.act, lambda e: e.activation(out=e1n[:], in_=bk[0][:], func=AF.Exp, scale=-1.0), [bk[0]], [e1n])
            P.op(P.act, lambda e: e.activation(out=e3[:], in_=bk[1][:], func=AF.Exp), [bk[1]], [e3])
            P.op(P.dve, lambda e: e.tensor_tensor(out=qt[:], in0=hq[:, cs_], in1=e1[:], op=ALU.mult), [hq, e1], [qt])
            P.op(P.dve, lambda e: e.tensor_tensor(out=kt[:], in0=kk[:, cs_], in1=e1n[:], op=ALU.mult), [kk, e1n], [kt])
            P.op(P.dve, lambda e: e.tensor_tensor(out=qh[:], in0=hq[:, cs_], in1=e3[:], op=ALU.mult), [hq, e3], [qh])
            T1 = bk[3][:].bitcast(BF16)
            T2 = bk[4][:].bitcast(BF16)
            for hh in range(4):
                hs = slice(hh * 128, (hh + 1) * 128)
                P.op(P.pe, lambda e: e.transpose(out=T1[:, hs], in_=qt[:, hs], identity=self.ident_bf[:]),
                     [qt, self.ident_bf], [bk[3]], inc=False)
            for hh in range(4):
                hs = slice(hh * 128, (hh + 1) * 128)
                P.op(P.pe, lambda e: e.transpose(out=T1[:, 512 + hh * 128:512 + (hh + 1) * 128], in_=kt[:, hs],
                                                 identity=self.ident_bf[:]), [kt, self.ident_bf], [bk[3]], inc=(hh == 3))
            for hh in range(4):
                hs = slice(hh * 128, (hh + 1) * 128)
                P.op(P.pe, lambda e: e.transpose(out=T2[:, hs], in_=qh[:, hs], identity=self.ident_bf[:]),
                     [qh, self.ident_bf], [bk[4]], inc=(hh == 3))
            P.op(P.act, lambda e: e.copy(out=qtT[:], in_=T1[:, 0:512]), [bk[3]], [qtT])
            P.op(P.act, lambda e: e.copy(out=ktT[:], in_=T1[:, 512:1024]), [bk[3]], [ktT])
            T2v = T2[:, 0:512].rearrange("p (h t) -> p h t", h=4)
            qf, qs, am = qhf[d][pu], qhs[d][pu], Am[d][pu]
            P.op(P.dve, lambda e: e.tensor_copy(out=qf[:, :, fr], in_=T2v[:, :, fr]), [bk[4]], [qf])
            P.op(P.dve, lambda e: e.tensor_copy(out=qs[:, :, sr], in_=T2v[:, :, sr]), [bk[4]], [qs])
            for hh in range(4):
                hs = slice(hh * 128, (hh + 1) * 128)
                P.op(P.pe, lambda e: e.matmul(bk[5][:, hs], ktT[:, hs], qtT[:, hs], start=True, stop=True),
                     [ktT, qtT], [bk[5]], inc=(hh == 3))
            P.op(P.dve, lambda e: e.copy_predicated(
                out=am[:], mask=mask[d][:].unsqueeze(1).to_broadcast([128, 4, 128]),
                data=bk[5][:].rearrange("p (h t) -> p h t", h=4)), [bk[5], mask[d]], [am])

        def back(u, idx, half, d, row0, srcK, srcLF, full, outd):
            L = ld[idx % 2]
            hi = L["hi"]
            ost = ostage[idx % 2]
            fc, sc = (0, 1) if d == 0 else (1, 0)
            fr = slice(fc * 64, fc * 64 + 64)
            sr = slice(sc * 64, sc * 64 + 64)
            pu = u % 2
            c0 = half * 512
            k_h, de = kh[pu], dec[pu]
            qf, qs, am = qhf[d][pu], qhs[d][pu], Am[d][pu]
            HS = [slice(hh * 128, (hh + 1) * 128) for hh in range(4)]
            HC = [slice(c0 + hh * 128, c0 + (hh + 1) * 128) for hh in range(4)]
            for hh in range(4):
                P.op(P.pe, lambda e: e.matmul(bk[7][:, HS[hh]], k_h[fr, HS[hh]], hi[fr, HC[hh]], start=True, stop=True),
                     [k_h, hi], [bk[7]], inc=False)
                P.op(P.pe, lambda e: e.matmul(bk[2][:, HS[hh]], k_h[sr, HS[hh]], hi[sr, HC[hh]], start=True, stop=True),
                     [k_h, hi], [bk[2]], inc=(hh == 3))
            if full:
                for hh in range(4):
                    h = half * 4 + hh
                    P.op(P.pe, lambda e: e.matmul(bk[6][:, HS[hh]], am[:, hh, :], hi[:, HC[hh]], start=True, stop=False),
                         [am, hi], [bk[6]], inc=False)
                    P.op(P.pe, lambda e: e.matmul(bk[6][:, HS[hh]], qf[:, hh, :], Sb[h][:], start=False, stop=True),
                         [qf, Sb[h]], [bk[6]])
            for hh in range(4):
                h = half * 4 + hh
                P.op(P.dve, lambda e: e.scalar_tensor_tensor(out=S[h][:], in0=S[h][:], scalar=de[:, hh * 2 + fc:hh * 2 + fc + 1],
                                                             in1=bk[7][:, HS[hh]], op0=ALU.mult, op1=ALU.add),
                     [S[h], de, bk[7]], [S[h]])
            for hh in range(4):
                h = half * 4 + hh
                P.op(P.act, lambda e: e.copy(out=Sb[h][:], in_=S[h][:]), [S[h]], [Sb[h]])
            if full:
                for hh in range(4):
                    h = half * 4 + hh
                    P.op(P.pe, lambda e: e.matmul(bk[0][:, HS[hh]], qs[:, hh, :], Sb[h][:], start=True, stop=True),
                         [qs, Sb[h]], [bk[0]])
            for hh in range(4):
                h = half * 4 + hh
                P.op(P.dve, lambda e: e.scalar_tensor_tensor(out=S[h][:], in0=S[h][:], scalar=de[:, hh * 2 + sc:hh * 2 + sc + 1],
                                                             in1=bk[2][:, HS[hh]], op0=ALU.mult, op1=ALU.add),
                     [S[h], de, bk[2]], [S[h]])
            for hh in range(4):
                h = half * 4 + hh
                P.op(P.act, lambda e: e.copy(out=Sb[h][:], in_=S[h][:]), [S[h]], [Sb[h]])
            if full:
                P.op(P.act, lambda e: e.copy(out=ost[:, c0:c0 + 512], in_=bk[6][:]), [bk[6]], [ost])
                P.op(P.dve, lambda e: e.tensor_tensor(out=ost[:, c0:c0 + 512], in0=ost[:, c0:c0 + 512], in1=bk[0][:], op=ALU.add),
                     [ost, bk[0]], [ost])
                if half == 1:
                    P.dma(P.sp, outd, outd[row0:row0 + 128, :], ost, ost[:])

        ctx_full = not last
        plan.append(("reset",))
        for i in (16, 17):
            tile(0, i * 128, self.KA, self.LFA, ctx_full, self.OA)
        for i in range(16):
            tile(0, i * 128, self.KA, self.LFA, True, self.OA)
        if full:
            for j in range(15, -1, -1):
                tile(1, NTOK + j * 128, self.KA, self.LFA, True, self.OB)
        plan.append(("reset",))
        for i in (17, 16):
            tile(1, i * 128, self.KB, self.LFB, ctx_full, self.OB)
        for j in range(16):
            tile(0, NTOK + j * 128, self.KB, self.LFB, full, self.OA)
        for i in range(15, -1, -1):
            tile(1, i * 128, self.KB, self.LFB, True, self.OB)

        units = []
        idx = 0
        pending_reset = False
        for p in plan:
            if p[0] == "reset":
                pending_reset = True
                continue
            for half in range(2):
                units.append((pending_reset and half == 0, idx, half, p[1:]))
            pending_reset = False
            idx += 1
        tl = [p[1:] for p in plan if p[0] == "tile"]
        tile_loads(0, *tl[0])
        if len(tl) > 1:
            tile_loads(1, *tl[1])
        front(0, units[0][1], units[0][2], *units[0][3])
        for u, (rst, idx, half, args) in enumerate(units):
            if u + 1 < len(units):
                _, i2, h2, a2 = units[u + 1]
                front(u + 1, i2, h2, *a2)
            if rst:
                reset_state()
            back(u, idx, half, *args)
            if half == 1 and idx + 2 < len(tl):
                tile_loads(idx + 2, *tl[idx + 2])
        P.end()

    def resid(self, zb, xt, xidx, gi, xo, ss, tmp):
        P = self.P
        junk = tmp
        for hf in range(2):
            P.op(P.act, lambda e: e.activation(out=junk[:, 0:512], in_=zb[hf][1], func=AF.Square, accum_out=ss[:, hf:hf + 1]),
                 [zb[hf][0]], [junk, ss])
        P.op(P.dve, lambda e: e.tensor_tensor(out=ss[:, 2:3], in0=ss[:, 0:1], in1=ss[:, 1:2], op=ALU.add), [ss], [ss])
        P.op(P.act, lambda e: e.activation(out=ss[:, 3:4], in_=ss[:, 2:3], func=AF.Sqrt, scale=1.0 / D, bias=EPS), [ss], [ss])
        P.op(P.dve, lambda e: e.reciprocal(out=ss[:, 2:3], in_=ss[:, 3:4]), [ss], [ss])
        for hf in range(2):
            cs_ = slice(hf * 512, (hf + 1) * 512)
            P.op(P.dve, lambda e: e.tensor_scalar(out=tmp[:, cs_], in0=zb[hf][1], scalar1=ss[:, 2:3], scalar2=None, op0=ALU.mult),
                 [zb[hf][0], ss], [tmp])
            P.op(P.dve, lambda e: e.tensor_tensor(out=tmp[:, cs_], in0=tmp[:, cs_], in1=self.grow[:, xidx, gi, cs_], op=ALU.mult),
                 [tmp, self.grow], [tmp])
            P.op(P.dve, lambda e: e.tensor_tensor(out=xo[:, cs_], in0=tmp[:, cs_], in1=xt[:, cs_], op=ALU.add), [tmp, xt], [xo])

    def phase_X(self, layer, tiles, xsrc, xdst):
        P = self.P
        P.begin()
        W = {}
        for nm, src in (("att", self.w_att), ("hg", self.w_hg), ("out", self.w_out)):
            W[nm] = P.sb([128, 8, D], BF16, "W" + nm)
            P.dma(P.pool, W[nm], W[nm][:], src, src[layer].rearrange("(kc k) j -> k kc j", k=128))
        hgn = P.sb([128, 128], F32, "hgn")
        P.dma(P.sp, hgn, hgn[:], self.rowv, self.rowv[layer][:, 4352:4480])
        L = [dict(oa=P.sb([128, D], F32, "oa%d" % i), ob=P.sb([128, D], F32, "ob%d" % i),
                  hg=P.sb([128, D], BF16, "hg%d" % i), ga=P.sb([128, D], BF16, "ga%d" % i),
                  gh=P.sb([128, D], BF16, "gh%d" % i), at=P.sb([128, 8, 128], BF16, "at%d" % i),
                  x=P.sb([128, D], F32, "x%d" % i)) for i in range(3)]
        junk = P.sb([128, D], F32, "junkX")
        junk2 = P.sb([128, D], F32, "junkX2")
        ss = P.sb([128, 16], F32, "ssX")
        ss2 = P.sb([128, 4], F32, "ss2X")
        hgo = [P.sb([128, D], BF16, "hgo%d" % i) for i in range(2)]
        hgT = [P.sb([128, 8, 128], BF16, "hgT%d" % i) for i in range(2)]
        y1 = P.sb([128, 512], F32, "y1")
        y2 = P.sb([128, 512], F32, "y2")
        ybf = [P.sb([128, D], BF16, "ybf%d" % i) for i in range(2)]
        yT = [P.sb([128, 8, 128], BF16, "yT%d" % i) for i in range(2)]
        xo = [P.sb([128, D], F32, "xo%d" % i) for i in range(2)]
        bk = self.bank
        NT = len(tiles)

        def loads(it):
            n = tiles[it]
            l = L[it % 3]
            rs = slice(n * 128, (n + 1) * 128)
            P.dma(P.sp, l["oa"], l["oa"][:], self.OA, self.OA[rs, :])
            P.dma(P.sp, l["ob"], l["ob"][:], self.OB, self.OB[rs, :])
            P.dma(P.sp, l["hg"], l["hg"][:], self.HG, self.HG[rs, :])
            P.dma(P.sp, l["ga"], l["ga"][:], self.GA, self.GA[rs, :])
            P.dma(P.sp, l["gh"], l["gh"][:], self.GH, self.GH[rs, :])
            P.dma(P.sp, l["at"], l["at"][:], self.ATT, self.ATT[:, :, n * 128:(n + 1) * 128].rearrange("h d t -> d h t"))
            xb, xr = xsrc(n)
            P.dma(P.sp, l["x"], l["x"][:], xb, xb[xr:xr + 128, :])

        def readout(it):
            l = L[it % 3]
            o = l["oa"]
            hg_o = hgo[it % 2]
            P.op(P.pool, lambda e: e.tensor_tensor(out=o[:], in0=o[:], in1=l["ob"][:], op=ALU.add), [o, l["ob"]], [o])
            P.op(P.act, lambda e: e.activation(out=junk[:], in_=o[:], func=AF.Square), [o], [junk])
            P.op(P.dve, lambda e: e.tensor_reduce(out=ss[:, 0:8], in_=junk[:].rearrange("p (h d) -> p h d", h=8), axis=AX.X, op=ALU.add),
                 [junk], [ss])
            P.op(P.act, lambda e: e.activation(out=ss[:, 8:16], in_=ss[:, 0:8], func=AF.Sqrt, scale=1.0 / 128, bias=EPS), [ss], [ss])
            P.op(P.dve, lambda e: e.reciprocal(out=ss[:, 0:8], in_=ss[:, 8:16]), [ss], [ss])
            o3 = o[:].rearrange("p (h d) -> p h d", h=8)
            P.op(P.dve, lambda e: e.tensor_tensor(out=o3, in0=o3, in1=ss[:, 0:8].unsqueeze(2).to_broadcast([128, 8, 128]), op=ALU.mult),
                 [o, ss], [o])
            P.op(P.dve, lambda e: e.tensor_tensor(out=o3, in0=o3, in1=hgn[:].unsqueeze(1).to_broadcast([128, 8, 128]), op=ALU.mult),
                 [o, hgn], [o])
            P.op(P.pool, lambda e: e.tensor_tensor(out=hg_o[:], in0=o[:], in1=l["hg"][:], op=ALU.mult), [o, l["hg"]], [hg_o])

        def branch_mm(it):
            l = L[it % 3]
            hg_o = hgo[it % 2]
            hg_T = hgT[it % 2]
            T0 = bk[0][:].bitcast(BF16)
            for kc in range(8):
                P.op(P.pe, lambda e: e.transpose(out=T0[:, kc * 128:(kc + 1) * 128], in_=hg_o[:, kc * 128:(kc + 1) * 128],
                                                 identity=self.ident_bf[:]), [hg_o, self.ident_bf], [bk[0]], inc=(kc == 7))
            P.op(P.act, lambda e: e.copy(out=hg_T[:].rearrange("p k t -> p (k t)"), in_=T0[:, :]), [bk[0]], [hg_T])
            for hf in range(2):
                cs_ = slice(hf * 512, (hf + 1) * 512)
                for kc in range(8):
                    P.op(P.pe, lambda e: e.matmul(bk[2 + hf][:], l["at"][:, kc, :], W["att"][:, kc, cs_], start=(kc == 0), stop=(kc == 7)),
                         [l["at"], W["att"]], [bk[2 + hf]], inc=(kc == 7))
                for kc in range(8):
                    P.op(P.pe, lambda e: e.matmul(bk[4 + hf][:], hg_T[:, kc, :], W["hg"][:, kc, cs_], start=(kc == 0), stop=(kc == 7)),
                         [hg_T, W["hg"]], [bk[4 + hf]], inc=(kc == 7))

        def gate_merge(it):
            l = L[it % 3]
            y_bf = ybf[it % 2]
            for hf in range(2):
                cs_ = slice(hf * 512, (hf + 1) * 512)
                P.op(P.dve, lambda e: e.tensor_tensor(out=y1[:], in0=bk[2 + hf][:], in1=l["ga"][:, cs_], op=ALU.mult),
                     [bk[2 + hf], l["ga"]], [y1])
                P.op(P.dve, lambda e: e.tensor_tensor(out=y2[:], in0=bk[4 + hf][:], in1=l["gh"][:, cs_], op=ALU.mult),
                     [bk[4 + hf], l["gh"]], [y2])
                P.op(P.dve, lambda e: e.tensor_tensor(out=y_bf[:, cs_], in0=y1[:], in1=y2[:], op=ALU.add), [y1, y2], [y_bf])

        def out_mm(it):
            y_bf = ybf[it % 2]
            y_T = yT[it % 2]
            T1 = bk[1][:].bitcast(BF16)
            for kc in range(8):
                P.op(P.pe, lambda e: e.transpose(out=T1[:, kc * 128:(kc + 1) * 128], in_=y_bf[:, kc * 128:(kc + 1) * 128],
                                                 identity=self.ident_bf[:]), [y_bf, self.ident_bf], [bk[1]], inc=(kc == 7))
            P.op(P.act, lambda e: e.copy(out=y_T[:].rearrange("p k t -> p (k t)"), in_=T1[:, :]), [bk[1]], [y_T])
            for hf in range(2):
                cs_ = slice(hf * 512, (hf + 1) * 512)
                for kc in range(8):
                    P.op(P.pe, lambda e: e.matmul(bk[6 + hf][:], y_T[:, kc, :], W["out"][:, kc, cs_], start=(kc == 0), stop=(kc == 7)),
                         [y_T, W["out"]], [bk[6 + hf]], inc=(kc == 7))

        def finish_tile(it):
            n = tiles[it]
            l = L[it % 3]
            xidx = 1 if n in (16, 17) else 0
            x_o = xo[it % 2]
            self.resid([(bk[6], bk[6][:]), (bk[7], bk[7][:])], l["x"], xidx, 0, x_o, ss2, junk2)
            P.dma(P.sp, xdst, xdst[n * 128:(n + 1) * 128, :], x_o, x_o[:])

        loads(0)
        if NT > 1:
            loads(1)
        readout(0)
        branch_mm(0)
        gate_merge(0)
        for it in range(NT):
            out_mm(it)
            if it + 2 < NT:
                loads(it + 2)
            if it + 1 < NT:
                readout(it + 1)
                branch_mm(it + 1)
            finish_tile(it)
            if it + 1 < NT:
                gate_merge(it + 1)
        P.end()

    def phase_F(self, xsrc_buf, xdst_fn, tiles, groups):
        P = self.P
        P.begin()
        wg = [P.sb([128, 8, 512], BF16, "wg%d" % i) for i in range(2)]
        wu = [P.sb([128, 8, 512], BF16, "wu%d" % i) for i in range(2)]
        sg = [P.sb([128, 512], F32, "sg%d" % i) for i in range(2)]
        ast = [P.sb([128, 512], BF16, "ast%d" % i) for i in range(3)]
        bk = self.bank
        cnt = 0
        nblk = (DFF + 511) // 512
        for fb in range(nblk):
            wdt = min(512, DFF - fb * 512)
            g_, u_ = wg[fb % 2], wu[fb % 2]
            P.dma(P.pool, g_, g_[:, :, :wdt], self.ffn_g, self.ffn_g[:, fb * 512:fb * 512 + wdt].rearrange("(kc k) j -> k kc j", k=128))
            P.dma(P.pool, u_, u_[:, :, :wdt], self.ffn_u, self.ffn_u[:, fb * 512:fb * 512 + wdt].rearrange("(kc k) j -> k kc j", k=128))
            for fcl in range(wdt // 128):
                fc = fb * 4 + fcl
                fs = slice(fcl * 128, (fcl + 1) * 128)
                for (col0, n) in groups:
                    pg = bk[cnt % 2]
                    pu = bk[2 + cnt % 2]
                    s_ = sg[cnt % 2]
                    a_ = ast[cnt % 3]
                    cnt += 1
                    for kc in range(8):
                        P.op(P.pe, lambda e: e.matmul(pg[:, :n], g_[:, kc, fs], self.hT[:, kc, col0:col0 + n], start=(kc == 0), stop=(kc == 7)),
                             [g_, self.hT], [pg], inc=(kc == 7))
                    for kc in range(8):
                        P.op(P.pe, lambda e: e.matmul(pu[:, :n], u_[:, kc, fs], self.hT[:, kc, col0:col0 + n], start=(kc == 0), stop=(kc == 7)),
                             [u_, self.hT], [pu], inc=(kc == 7))
                    P.op(P.act, lambda e: e.activation(out=s_[:, :n], in_=pg[:, :n], func=AF.Silu), [pg], [s_])
                    P.op(P.dve, lambda e: e.tensor_tensor(out=a_[:, :n], in0=s_[:, :n], in1=pu[:, :n], op=ALU.mult), [s_, pu], [a_])
                    P.dma(P.sp, self.AT, self.AT[fc, :, col0:col0 + n], a_, a_[:, :n])
        P.end()
        P.begin()
        NFC = DFF // 128
        wd = P.sb([128, NFC, D], BF16, "wd")
        P.dma(P.pool, wd, wd[:, 0:11, :], self.ffn_d, self.ffn_d[0:11 * 128, :].rearrange("(c f) j -> f c j", f=128))
        P.dma(P.pool, wd, wd[:, 11:22, :], self.ffn_d, self.ffn_d[11 * 128:22 * 128, :].rearrange("(c f) j -> f c j", f=128))
        aT = [P.sb([128, NFC, 128], BF16, "aT%d" % i) for i in range(2)]
        xt = [P.sb([128, D], F32, "xF%d" % i) for i in range(2)]
        xo = [P.sb([128, D], F32, "xoF%d" % i) for i in range(2)]
        tmp = P.sb([128, D], F32, "tmpF")
        ss = P.sb([128, 4], F32, "ssF")
        def loadsF(it):
            n = tiles[it]
            P.dma(P.sp, aT[it % 2], aT[it % 2][:], self.AT, self.AT[0:NFC, :, n * 128:(n + 1) * 128].rearrange("c f t -> f c t"))
            P.dma(P.sp, xt[it % 2], xt[it % 2][:], xsrc_buf, xsrc_buf[n * 128:(n + 1) * 128, :])

        loadsF(0)
        for it, n in enumerate(tiles):
            a_ = aT[it % 2]
            x_ = xt[it % 2]
            xidx = 1 if n in (16, 17) else 0
            rs = slice(n * 128, (n + 1) * 128)
            zb = [bk[4 + 2 * (it % 2)], bk[5 + 2 * (it % 2)]]
            for hf in range(2):
                for fc in range(NFC):
                    P.op(P.pe, lambda e: e.matmul(zb[hf][:], a_[:, fc, :], wd[:, fc, hf * 512:(hf + 1) * 512], start=(fc == 0), stop=(fc == NFC - 1)),
                         [a_, wd], [zb[hf]], inc=(fc == NFC - 1))
            if it + 1 < len(tiles):
                loadsF(it + 1)
            x_o = xo[it % 2]
            self.resid([(zb[0], zb[0][:]), (zb[1], zb[1][:])], x_, xidx, 1, x_o, ss, tmp)
            db, dr = xdst_fn(n)
            P.dma(P.sp, db, db[dr:dr + 128, :], x_o, x_o[:])
        P.end()

    def phase_R(self, xsrc_buf, gates):
        P = self.P
        P.begin()
        wr = P.sb([128, 8, NEXP], F32, "wr")
        P.dma(P.sp, wr, wr[:], self.moe_r, self.moe_r[:].rearrange("(kc k) e -> k kc e", k=128))
        xt = [P.sb([128, D], F32, "xR%d" % i) for i in range(2)]
        xs = P.sb([128, D], F32, "xsR")
        junk = P.sb([128, D], BF16, "junkR")
        fT = P.sb([128, 8, 128], F32, "fT32")
        ss = P.sb([128, 2], F32, "ssR")
        lg = P.sb([128, 8], F32, "lg")
        l2 = P.sb([128, 8], F32, "l2")
        eq1 = P.sb([128, 8], F32, "eq1")
        eq2 = P.sb([128, 8], F32, "eq2")
        sm = P.sb([128, 8], F32, "sm")
        bk = self.bank
        for n in range(16):
            a = xt[n % 2]
            P.dma(P.sp, a, a[:], xsrc_buf, xsrc_buf[n * 128:(n + 1) * 128, :])
            P.op(P.act, lambda e: e.activation(out=junk[:], in_=a[:], func=AF.Square, accum_out=ss[:, 0:1]), [a], [junk, ss])
            P.op(P.act, lambda e: e.activation(out=ss[:, 1:2], in_=ss[:, 0:1], func=AF.Sqrt, scale=1.0 / D, bias=EPS), [ss], [ss])
            P.op(P.dve, lambda e: e.reciprocal(out=ss[:, 0:1], in_=ss[:, 1:2]), [ss], [ss])
            P.op(P.dve, lambda e: e.tensor_scalar(out=xs[:], in0=a[:], scalar1=ss[:, 0:1], scalar2=None, op0=ALU.mult), [a, ss], [xs])
            for kc in range(8):
                pb = bk[kc // 4]
                P.op(P.pe, lambda e: e.transpose(out=pb[:, (kc % 4) * 128:(kc % 4 + 1) * 128], in_=xs[:, kc * 128:(kc + 1) * 128],
                                                 identity=self.ident_f), [xs, self.c_f32], [pb], inc=(kc % 4 == 3))
            for kc in range(8):
                pb = bk[kc // 4]
                P.op(P.dve, lambda e: e.tensor_scalar(out=fT[:, kc, :], in0=pb[:, (kc % 4) * 128:(kc % 4 + 1) * 128],
                                                      scalar1=self.AB[:, 2, kc, 0:1], scalar2=self.AB[:, 3, kc, 0:1],
                                                      op0=ALU.mult, op1=ALU.add), [pb, self.AB], [fT])
            for kc in range(8):
                P.op(P.pe, lambda e: e.matmul(bk[2][:, 0:NEXP], fT[:, kc, :], wr[:, kc, :], start=(kc == 0), stop=(kc == 7)),
                     [fT, wr], [bk[2]], inc=(kc == 7))
            P.op(P.dve, lambda e: e.tensor_copy(out=lg[:], in_=bk[2][:, 0:NEXP]), [bk[2]], [lg])
            P.op(P.dve, lambda e: e.tensor_reduce(out=sm[:, 0:1], in_=lg[:], axis=AX.X, op=ALU.max), [lg], [sm])
            P.op(P.dve, lambda e: e.tensor_scalar(out=eq1[:], in0=lg[:], scalar1=sm[:, 0:1], scalar2=None, op0=ALU.is_equal), [lg, sm], [eq1])
            P.op(P.dve, lambda e: e.scalar_tensor_tensor(out=l2[:], in0=eq1[:], scalar=-1e30, in1=lg[:], op0=ALU.mult, op1=ALU.add),
                 [eq1, lg], [l2])
            P.op(P.dve, lambda e: e.tensor_reduce(out=sm[:, 1:2], in_=l2[:], axis=AX.X, op=ALU.max), [l2], [sm])
            P.op(P.dve, lambda e: e.tensor_scalar(out=eq2[:], in0=l2[:], scalar1=sm[:, 1:2], scalar2=None, op0=ALU.is_equal), [l2, sm], [eq2])
            P.op(P.dve, lambda e: e.tensor_tensor(out=sm[:, 2:3], in0=sm[:, 1:2], in1=sm[:, 0:1], op=ALU.subtract), [sm], [sm])
            P.op(P.act, lambda e: e.activation(out=sm[:, 3:4], in_=sm[:, 2:3], func=AF.Exp), [sm], [sm])
            P.op(P.dve, lambda e: e.tensor_scalar(out=sm[:, 4:5], in0=sm[:, 3:4], scalar1=1.0, scalar2=None, op0=ALU.add), [sm], [sm])
            P.op(P.dve, lambda e: e.reciprocal(out=sm[:, 5:6], in_=sm[:, 4:5]), [sm], [sm])
            P.op(P.dve, lambda e: e.tensor_tensor(out=sm[:, 6:7], in0=sm[:, 3:4], in1=sm[:, 5:6], op=ALU.mult), [sm], [sm])
            P.op(P.dve, lambda e: e.tensor_scalar(out=eq1[:], in0=eq1[:], scalar1=sm[:, 5:6], scalar2=None, op0=ALU.mult), [eq1, sm], [eq1])
            P.op(P.dve, lambda e: e.scalar_tensor_tensor(out=gates[:, n, :], in0=eq2[:], scalar=sm[:, 6:7], in1=eq1[:], op0=ALU.mult, op1=ALU.add),
                 [eq2, sm, eq1], [gates])
        P.end()

    def phase_E1(self):
        P = self.P
        P.begin()
        wg = [P.sb([128, 8, 512], BF16, "wgE%d" % i) for i in range(2)]
        wu = [P.sb([128, 8, 512], BF16, "wuE%d" % i) for i in range(2)]
        sg = [P.sb([128, 512], F32, "sgE%d" % i) for i in range(2)]
        ast = [P.sb([128, 512], BF16, "astE%d" % i) for i in range(3)]
        bk = self.bank
        cnt = 0
        wc = 0
        for ex in range(NEXP):
            for fb in range(DFE // 512):
                g_, u_ = wg[wc % 2], wu[wc % 2]
                wc += 1
                P.dma(P.pool, g_, g_[:], self.moe_g, self.moe_g[ex][:, fb * 512:(fb + 1) * 512].rearrange("(kc k) j -> k kc j", k=128))
                P.dma(P.pool, u_, u_[:], self.moe_u, self.moe_u[ex][:, fb * 512:(fb + 1) * 512].rearrange("(kc k) j -> k kc j", k=128))
                for fcl in range(4):
                    fc = fb * 4 + fcl
                    fs = slice(fcl * 128, (fcl + 1) * 128)
                    for tg in range(4):
                        col0 = tg * 512
                        pg = bk[cnt % 2]
                        pu = bk[2 + cnt % 2]
                        s_ = sg[cnt % 2]
                        a_ = ast[cnt % 3]
                        cnt += 1
                        for kc in range(8):
                            P.op(P.pe, lambda e: e.matmul(pg[:], g_[:, kc, fs], self.hT[:, kc, col0:col0 + 512], start=(kc == 0), stop=(kc == 7)),
                                 [g_, self.hT], [pg], inc=(kc == 7))
                        for kc in range(8):
                            P.op(P.pe, lambda e: e.matmul(pu[:], u_[:, kc, fs], self.hT[:, kc, col0:col0 + 512], start=(kc == 0), stop=(kc == 7)),
                                 [u_, self.hT], [pu], inc=(kc == 7))
                        P.op(P.act, lambda e: e.activation(out=s_[:], in_=pg[:], func=AF.Silu), [pg], [s_])
                        P.op(P.dve, lambda e: e.tensor_tensor(out=a_[:], in0=s_[:], in1=pu[:], op=ALU.mult), [s_, pu], [a_])
                        P.dma(P.sp, self.ATE, self.ATE[ex, fc, :, col0:col0 + 512], a_, a_[:])
        P.end()

    def phase_E2(self, gates, xsrc_buf, xdst_fn):
        P = self.P
        P.begin()
        NFC = DFE // 128
        yacc = P.sb([128, 16, D], F32, "yacc")
        P.begin()
        HP = NFC // 2
        wd = [P.sb([128, HP, D], BF16, "wdE%d" % i) for i in range(3)]
        aT = [P.sb([128, HP, 128], BF16, "aTE%d" % i) for i in range(2)]
        bk = self.bank
        cnt = 0
        for ex in range(NEXP):
            for part in range(2):
                w = wd[(ex * 2 + part) % 3]
                for sp_ in range(2):
                    r0 = part * HP * 128 + sp_ * 7 * 128
                    P.dma(P.pool, w, w[:, sp_ * 7:(sp_ + 1) * 7, :], self.moe_d,
                          self.moe_d[ex][r0:r0 + 896, :].rearrange("(c f) j -> f c j", f=128))
                for n in range(16):
                    a_ = aT[cnt % 2]
                    zb = [bk[4 * (cnt % 2)], bk[4 * (cnt % 2) + 1]]
                    cnt += 1
                    P.dma(P.sp, a_, a_[:], self.ATE,
                          self.ATE[ex, part * HP:(part + 1) * HP, :, n * 128:(n + 1) * 128].rearrange("c f t -> f c t"))
                    for hf in range(2):
                        for fc in range(HP):
                            P.op(P.pe, lambda e: e.matmul(zb[hf][:], a_[:, fc, :], w[:, fc, hf * 512:(hf + 1) * 512], start=(fc == 0), stop=(fc == HP - 1)),
                                 [a_, w], [zb[hf]], inc=(fc == HP - 1))
                    for hf in range(2):
                        dst = yacc[:, n, hf * 512:(hf + 1) * 512]
                        if ex == 0 and part == 0:
                            P.op(P.dve, lambda e: e.tensor_scalar(out=dst, in0=zb[hf][:], scalar1=gates[:, n, ex:ex + 1], scalar2=None, op0=ALU.mult),
                                 [zb[hf], gates], [yacc])
                        else:
                            P.op(P.dve, lambda e: e.scalar_tensor_tensor(out=dst, in0=zb[hf][:], scalar=gates[:, n, ex:ex + 1], in1=dst,
                                                                         op0=ALU.mult, op1=ALU.add), [zb[hf], gates, yacc], [yacc])
        P.end()
        P.begin()
        xt = [P.sb([128, D], F32, "xE%d" % i) for i in range(2)]
        xo = [P.sb([128, D], F32, "xoE%d" % i) for i in range(2)]
        tmp = P.sb([128, D], F32, "tmpE")
        ss = P.sb([128, 4], F32, "ssE")
        for n in range(16):
            x_ = xt[n % 2]
            x_o = xo[n % 2]
            P.dma(P.sp, x_, x_[:], xsrc_buf, xsrc_buf[n * 128:(n + 1) * 128, :])
            self.resid([(yacc, yacc[:, n, 0:512]), (yacc, yacc[:, n, 512:1024])], x_, 0, 1, x_o, ss, tmp)
            db, dr = xdst_fn(n)
            P.dma(P.sp, db, db[dr:dr + 128, :], x_o, x_o[:])
        P.end()
        P.end()

    def layer(self, layer, last, xsrc, xdst):
        P = self.P
        full = not last
        PAR_BLK = {2, 7, 8, 9, 10}
        htiles = [(xsrc(n)[0], xsrc(n)[1], 1 if n in (16, 17) else 0, n * 128) for n in range(34)]
        ptiles = []
        for n in range(34):
            if n < 16:
                ptiles.append((n * 128, self.cs_own, n * 128, None))
            elif n < 18:
                ptiles.append((n * 128, None, 0, None if not last else {2, 5, 6, 7, 8, 9, 10}))
            else:
                ptiles.append((n * 128, self.cs_par, (n - 18) * 128, None if full else PAR_BLK))
        self.phase_M(layer)
        P.begin()
        self.hT = P.sb([128, 8, NKEY], BF16, "hT")
        self.phase_H(htiles, 0)
        self.phase_P(layer, ptiles)
        P.end()
        groups = [(gi * 512, 512, list(range(34))) for gi in range(4)]
        if not last:
            groups.append((NLAT, 256, [16, 17]))
        if full:
            groups += [(NTOK + gi * 512, 512, list(range(34))) for gi in range(4)]
        self.phase_A(layer, groups)
        self.phase_G(layer, last, full)
        act = list(range(16)) + ([16, 17] if not last else []) + (list(range(18, 34)) if full else [])
        self.phase_X(layer, act, xsrc, self.X1)
        if layer % 2 == 0:
            P.begin()
            self.hT = P.sb([128, 8, NKEY], BF16, "hT")
            self.phase_H([(self.X1, n * 128, 1 if n in (16, 17) else 0, n * 128) for n in act], 1)
            fgroups = [(g * 512, 512) for g in range(4)]
            if not last:
                fgroups.append((NLAT, 256))
            if full:
                fgroups += [(NTOK + g * 512, 512) for g in range(4)]
            self.phase_F(self.X1, xdst, act, fgroups)
            P.end()
        else:
            gates = P.sb([128, 16, NEXP], F32, "gates")
            self.phase_R(self.X1, gates)
            P.begin()
            self.hT = P.sb([128, 8, NLAT], BF16, "hT")
            self.phase_H([(self.X1, i * 128, 0, i * 128) for i in range(16)], 1)
            self.phase_E1()
            P.end()
            self.phase_E2(gates, self.X1, xdst)

    def finish(self):
        P = self.P
        P.barrier()
        return self.nc


def own_tiles(xl, xc):
    t = [(xl, i * 128, 0, i * 128) for i in range(16)]
    t += [(xc, i * 128, 1, NLAT + i * 128) for i in range(2)]
    return t


def _rope_tables(pos):
    pos = np.asarray(pos)
    rows = (pos // 64).astype(np.float32)
    cols = (pos % 64).astype(np.float32)
    inv = (np.float32(10000.0) ** (-np.arange(32, dtype=np.float32) / np.float32(32))).astype(np.float32)
    ang = np.concatenate([rows[:, None] * inv, cols[:, None] * inv], axis=-1).astype(np.float32)
    ang = np.concatenate([ang, ang], axis=-1)
    cos = np.cos(ang).astype(np.float32)
    sin = np.sin(ang).astype(np.float32)
    sin[:, :64] *= -1.0
    return np.concatenate([cos, sin], axis=-1).astype(np.float32)


def _consts():
    c = np.zeros((128, 1024), np.float32)
    c[:, 0:128] = np.eye(128, dtype=np.float32)
    s = np.arange(128)[:, None]
    t = np.arange(128)[None, :]
    same = (s // 64) == (t // 64)
    ch0 = (t // 64) * 64
    for d, off in ((0, 128), (1, 512)):
        if d == 0:
            L = same & (s <= t)
            Lmid = same & (s <= ch0 + 31)
        else:
            L = same & (s >= t)
            Lmid = same & (s >= ch0 + 32)
        Ll = same
        c[:, off:off + 128] = L.astype(np.float32) - Lmid.astype(np.float32)
        c[:, off + 128:off + 256] = Ll.astype(np.float32) - L.astype(np.float32)
        c[:, off + 256:off + 384] = L.astype(np.float32)
    c[0:64, 896] = 1.0
    c[64:128, 897] = 1.0
    return c


def _rep(v):
    return np.broadcast_to(np.asarray(v, np.float32)[None, :], (128, v.shape[0]))


def _colchunks(v):
    return np.asarray(v, np.float32).reshape(8, 128).T


def prep_inputs(inp):
    x = inp["x"]; c = inp["c"]; ctx = inp["ctx"]; c_ctx = inp["c_ctx"]
    L = 2
    colv = np.zeros((L, 128, 48), np.float32)
    rowv = np.zeros((L, 128, 6528), np.float32)
    for l in range(L):
        bm = inp["b_mod"][l]
        colv[l, :, 0:8] = _colchunks(inp["pre_mix_norm"][l])
        colv[l, :, 8:16] = _colchunks(inp["pre_ffn_norm"][l])
        colv[l, :, 16:24] = _colchunks(bm[1024:2048])
        colv[l, :, 24:32] = _colchunks(bm[0:1024])
        colv[l, :, 32:40] = _colchunks(bm[4096:5120])
        colv[l, :, 40:48] = _colchunks(bm[3072:4096])
        rowv[l, :, 0:1024] = _rep(bm[2048:3072])
        rowv[l, :, 1024:2048] = _rep(bm[5120:6144])
        rowv[l, :, 2048:3072] = _rep(inp["post_mix_norm"][l])
        rowv[l, :, 3072:4096] = _rep(inp["post_ffn_norm"][l])
        rowv[l, :, 4096:4224] = _rep(inp["q_norm"][l])
        rowv[l, :, 4224:4352] = _rep(inp["k_norm"][l])
        rowv[l, :, 4352:4480] = _rep(inp["hg_norm"][l])
        rowv[l, :, 4480:5504] = _rep(inp["hg_lb_logits"][0])
        rowv[l, :, 5504:6528] = _rep(inp["hg_lb_logits"][1])
    consts = _consts()
    w_in0 = np.ascontiguousarray(inp["w_in"], np.float32)
    w_in1 = w_in0.copy()
    w_in1[:, :, 2560:3584] = w_in0[:, :, 3584:4608]
    w_in1[:, :, 3584:4608] = w_in0[:, :, 2560:3584]
    pos0 = np.arange(0, 2048)
    pos1 = np.arange(4095, 2047, -1)
    cs = [_rope_tables(pos0), _rope_tables(pos1)]
    shared = dict(
        consts=consts, w_mod=np.ascontiguousarray(inp["w_mod"], np.float32), colv=colv, rowv=rowv,
        w_att=np.ascontiguousarray(inp["w_att_branch"], np.float32),
        w_hg=np.ascontiguousarray(inp["w_hg_branch"], np.float32),
        w_out=np.ascontiguousarray(inp["w_out"], np.float32),
        ffn_g=np.ascontiguousarray(inp["ffn_w_gate"][0], np.float32),
        ffn_u=np.ascontiguousarray(inp["ffn_w_up"][0], np.float32),
        ffn_d=np.ascontiguousarray(inp["ffn_w_down"][0], np.float32),
        moe_r=np.ascontiguousarray(inp["moe_router"][0], np.float32),
        moe_g=np.ascontiguousarray(inp["moe_w_gate"][0], np.float32),
        moe_u=np.ascontiguousarray(inp["moe_w_up"][0], np.float32),
        moe_d=np.ascontiguousarray(inp["moe_w_down"][0], np.float32),
    )
    maps = []
    for core in range(8):
        b, half = core // 2, core % 2
        halves = [np.ascontiguousarray(x[b, :2048]), np.ascontiguousarray(x[b, 2048:][::-1])]
        cT = np.zeros((128, 16), np.float32)
        cT[:, 0::2] = _colchunks(c[b])
        cT[:, 1::2] = _colchunks(c_ctx)
        m = dict(shared)
        m.update(
            xl=halves[half], xp=halves[1 - half],
            xc=np.ascontiguousarray(ctx[b] if half == 0 else ctx[b][::-1]),
            cT=cT, cs_own=cs[half], cs_par=cs[1 - half],
            w_in=w_in0 if half == 0 else w_in1,
        )
        maps.append(m)
    return maps


def _run(nc, names, maps):
    ins = [{k: m[k] for k in names} for m in maps]
    res = run_bass_kernel_spmd(nc, ins, core_ids=list(range(8)))
    return res.results


def build_fused():
    b = Builder(moe=True, ctx_out=False)

    def src0(n):
        if n < 16:
            return (b.xl, n * 128)
        if n < 18:
            return (b.xc, (n - 16) * 128)
        return (b.xp, (n - 18) * 128)

    b.layer(0, False, src0, lambda n: (b.X2, n * 128))
    b.layer(1, True, lambda n: (b.X2, n * 128), lambda n: (b.yout, n * 128))
    return b


def kernel(**inputs):
    inputs = {k: np.asarray(v) for k, v in inputs.items()}
    maps = prep_inputs(inputs)
    b = build_fused()
    r = _run(b.finish(), b.in_names, maps)
    out = np.empty((4, 4096, D), np.float32)
    for bb in range(4):
        out[bb, :2048] = r[2 * bb]["yout"]
        out[bb, 2048:] = r[2 * bb + 1]["yout"][::-1]
    return out
```
